# Optimizing a Trainium2 kernel written in Bass

```python
import math
import jax, jax.numpy as jnp
from jax import lax
import numpy as np

D_MODEL = 1024
BATCH = 8
SEQ = 4096
DEPTH = 2

N_BRANCH = 4
BRANCH_W = 512
A_GROUPS = 4
A_CHUNK = 128
A_GROUP_W = BRANCH_W // A_GROUPS
B_HEADS = 4
B_HEAD_DIM = BRANCH_W // B_HEADS
MOBA_BLOCK = 256
MOBA_TOP_K = 3
MOBA_QUERY_CHUNK = 32
C_HEADS = 4
C_KEY_W = BRANCH_W // 2
C_HEAD_K = C_KEY_W // C_HEADS
C_HEAD_V = BRANCH_W // C_HEADS
GLA_RANK = 16
GLA_TAU = 16.0
GLA_CHUNK = 64
CONV_W = 3
EPS = 1e-6

A_COLS = 3 * BRANCH_W
B_COLS = 4 * BRANCH_W
C_COLS = 2 * C_KEY_W + 2 * BRANCH_W + GLA_RANK
D_COLS = 4 * BRANCH_W
A_OFF = 0
B_OFF = A_OFF + A_COLS
C_OFF = B_OFF + B_COLS
D_OFF = C_OFF + C_COLS
N_IN_COLS = D_OFF + D_COLS

kernel_name = "hybrid_gated_parallel_mixers"


def rms_norm(x, g):
    xf = x.astype(jnp.float32)
    y = xf * lax.rsqrt(jnp.mean(xf * xf, axis=-1, keepdims=True) + EPS)
    return (y * g.astype(jnp.float32)).astype(x.dtype)


def layer_norm(x, g, b):
    xf = x.astype(jnp.float32)
    mu = jnp.mean(xf, axis=-1, keepdims=True)
    var = jnp.mean(jnp.square(xf - mu), axis=-1, keepdims=True)
    y = (xf - mu) * lax.rsqrt(var + EPS)
    return (y * g.astype(jnp.float32) + b.astype(jnp.float32)).astype(x.dtype)


def alibi_slopes(n_heads):
    return jnp.exp2(-8.0 * jnp.arange(1, n_heads + 1, dtype=jnp.float32) / n_heads)


def spatial_gating_branch(p, ln_g, ln_b, ws, bs):
    bsz, s, _ = p.shape
    u, v, z = jnp.split(p, 3, axis=-1)
    u = jax.nn.gelu(u)
    v = layer_norm(jax.nn.gelu(v), ln_g, ln_b)
    nc = s // A_CHUNK
    v = v.reshape(bsz, nc, A_CHUNK, A_GROUPS, A_GROUP_W)
    causal = jnp.tril(jnp.ones((A_CHUNK, A_CHUNK), dtype=bool))
    w = jnp.where(causal[None], ws, jnp.zeros_like(ws))
    mixed = jnp.einsum('gts,bcsgd->bctgd', w, v) + bs.T[:, :, None]
    mixed = mixed.reshape(bsz, s, BRANCH_W)
    return u * mixed * jax.nn.silu(z)


def moba_branch(p, qn_g, kn_g):
    bsz, s, _ = p.shape
    hd = B_HEAD_DIM
    q, k, v, z = jnp.split(p, 4, axis=-1)
    q = rms_norm(q.reshape(bsz, s, B_HEADS, hd), qn_g)
    k = rms_norm(k.reshape(bsz, s, B_HEADS, hd), kn_g)
    v = v.reshape(bsz, s, B_HEADS, hd)
    nblk = -(-s // MOBA_BLOCK)
    s_pad = nblk * MOBA_BLOCK
    pad = ((0, 0), (0, s_pad - s), (0, 0), (0, 0))
    q, k, v = [jnp.pad(t, pad).transpose(0, 2, 1, 3) for t in (q, k, v)]
    kb = k.reshape(bsz, B_HEADS, nblk, MOBA_BLOCK, hd)
    vb = v.reshape(bsz, B_HEADS, nblk, MOBA_BLOCK, hd)
    k_mean = jnp.mean(kb.astype(jnp.float32), axis=3)

    pos = jnp.arange(s_pad)
    q_blk = pos // MOBA_BLOCK
    gate = jnp.einsum('bhtd,bhnd->bhtn', q.astype(jnp.float32), k_mean)
    fully_past = jnp.arange(nblk)[None, :] < q_blk[:, None]
    gate = jnp.where(fully_past, gate, -jnp.inf)
    n_sel = min(MOBA_TOP_K, nblk)
    _, sel = lax.top_k(gate, n_sel)
    sel_valid = sel < q_blk[:, None]

    scale = hd ** -0.5
    slopes = alibi_slopes(B_HEADS)
    qc_len = MOBA_QUERY_CHUNK
    bi = jnp.arange(bsz)[:, None, None, None]
    hi = jnp.arange(B_HEADS)[None, :, None, None]

    def attend_chunk(c):
        t0 = c * qc_len
        qc = lax.dynamic_slice_in_dim(q, t0, qc_len, axis=2)
        sel_c = lax.dynamic_slice_in_dim(sel, t0, qc_len, axis=2)
        valid_c = lax.dynamic_slice_in_dim(sel_valid, t0, qc_len, axis=2)
        t_pos = t0 + jnp.arange(qc_len)
        blk = t0 // MOBA_BLOCK
        k_sel = kb[bi, hi, sel_c]
        v_sel = vb[bi, hi, sel_c]
        k_pos_sel = sel_c[..., None] * MOBA_BLOCK + jnp.arange(MOBA_BLOCK)
        s_sel = jnp.einsum('bhqd,bhqnkd->bhqnk', qc, k_sel,
                           preferred_element_type=jnp.float32) * scale
        s_sel = s_sel - slopes[:, None, None, None] * (t_pos[:, None, None] - k_pos_sel)
        s_sel = jnp.where(valid_c[..., None], s_sel, -jnp.inf)
        k_own = lax.dynamic_index_in_dim(kb, blk, axis=2, keepdims=False)
        v_own = lax.dynamic_index_in_dim(vb, blk, axis=2, keepdims=False)
        k_pos_own = blk * MOBA_BLOCK + jnp.arange(MOBA_BLOCK)
        dist_own = t_pos[:, None] - k_pos_own[None, :]
        s_own = jnp.einsum('bhqd,bhkd->bhqk', qc, k_own,
                           preferred_element_type=jnp.float32) * scale
        s_own = s_own - slopes[:, None, None] * dist_own
        s_own = jnp.where(dist_own >= 0, s_own, -jnp.inf)
        scores = jnp.concatenate([s_sel.reshape(bsz, B_HEADS, qc_len, -1), s_own], axis=-1)
        probs = jax.nn.softmax(scores, axis=-1).astype(v.dtype)
        p_sel = probs[..., :n_sel * MOBA_BLOCK].reshape(bsz, B_HEADS, qc_len, n_sel, MOBA_BLOCK)
        p_own = probs[..., n_sel * MOBA_BLOCK:]
        return (jnp.einsum('bhqnk,bhqnkd->bhqd', p_sel, v_sel)
                + jnp.einsum('bhqk,bhkd->bhqd', p_own, v_own))

    out = lax.map(attend_chunk, jnp.arange(s_pad // qc_len))
    out = out.transpose(1, 0, 3, 2, 4).reshape(bsz, s_pad, BRANCH_W)[:, :s]
    return out * jax.nn.silu(z)


def gla_branch(p, w2, b2, on_g):
    bsz, s, _ = p.shape
    splits = [C_KEY_W, 2 * C_KEY_W, 2 * C_KEY_W + BRANCH_W, 2 * C_KEY_W + BRANCH_W + GLA_RANK]
    q, k, v, lr, z = jnp.split(p, splits, axis=-1)
    log_a = jax.nn.log_sigmoid((lr @ w2 + b2).astype(jnp.float32)) / GLA_TAU
    nc = s // GLA_CHUNK

    def to_chunks(t, dh):
        return t.astype(jnp.float32).reshape(bsz, nc, GLA_CHUNK, C_HEADS, dh).transpose(1, 0, 3, 2, 4)

    qc = to_chunks(q, C_HEAD_K) * (C_HEAD_K ** -0.5)
    kc = to_chunks(k, C_HEAD_K)
    vc = to_chunks(v, C_HEAD_V)
    gc = to_chunks(log_a, C_HEAD_K)
    causal = jnp.tril(jnp.ones((GLA_CHUNK, GLA_CHUNK), dtype=bool))

    def step(state, inp):
        qi, ki, vi, gi = inp
        b = jnp.cumsum(gi, axis=2)
        o_inter = jnp.einsum('bhtd,bhdv->bhtv', qi * jnp.exp(b), state)
        rel = b[:, :, :, None, :] - b[:, :, None, :, :]
        decay = jnp.exp(jnp.where(causal[:, :, None], rel, -jnp.inf))
        attn = jnp.einsum('bhtd,bhsd,bhtsd->bhts', qi, ki, decay)
        o_intra = jnp.einsum('bhts,bhsv->bhtv', attn, vi)
        b_last = b[:, :, -1:, :]
        k_dec = ki * jnp.exp(b_last - b)
        state = (state * jnp.exp(b_last[:, :, 0, :])[..., None]
                 + jnp.einsum('bhsd,bhsv->bhdv', k_dec, vi))
        return state, o_inter + o_intra

    state0 = jnp.zeros((bsz, C_HEADS, C_HEAD_K, C_HEAD_V), jnp.float32)
    _, o = lax.scan(step, state0, (qc, kc, vc, gc))
    o = o.transpose(1, 0, 3, 2, 4).reshape(bsz, s, C_HEADS, C_HEAD_V)
    o = rms_norm(o, on_g).reshape(bsz, s, BRANCH_W).astype(p.dtype)
    return o * jax.nn.silu(z)


def short_conv_branch(p, conv_w, conv_b):
    bg, cg, xin, z = jnp.split(p, 4, axis=-1)
    u = cg * xin
    y = lax.conv_general_dilated(u, conv_w[:, None, :].astype(u.dtype), window_strides=(1,),
                                 padding=[(CONV_W - 1, 0)],
                                 dimension_numbers=('NWC', 'WIO', 'NWC'),
                                 feature_group_count=BRANCH_W) + conv_b
    return bg * y * jax.nn.silu(z)


def setup_inputs(seed: int = 0) -> dict:
    key = jax.random.key(seed)
    ks = jax.random.split(key, 20)

    def nrm(k, shape, scale):
        return jax.random.normal(k, shape, jnp.float32) * scale

    L = DEPTH
    return {
        "x": nrm(ks[0], (BATCH, SEQ, D_MODEL), 1.0),
        "norm_g": 1.0 + nrm(ks[1], (L, D_MODEL), 0.02),
        "w_in": nrm(ks[2], (L, D_MODEL, N_IN_COLS), D_MODEL ** -0.5),
        "a_ln_g": 1.0 + nrm(ks[3], (L, BRANCH_W), 0.02),
        "a_ln_b": nrm(ks[4], (L, BRANCH_W), 0.02),
        "a_spatial_w": nrm(ks[5], (L, A_GROUPS, A_CHUNK, A_CHUNK), A_CHUNK ** -0.5),
        "a_spatial_b": 1.0 + nrm(ks[6], (L, A_GROUPS, A_CHUNK), 0.02),
        "b_q_norm_g": 1.0 + nrm(ks[7], (L, B_HEAD_DIM), 0.02),
        "b_k_norm_g": 1.0 + nrm(ks[8], (L, B_HEAD_DIM), 0.02),
        "c_gate_w2": nrm(ks[9], (L, GLA_RANK, C_KEY_W), GLA_RANK ** -0.5),
        "c_gate_b": nrm(ks[10], (L, C_KEY_W), 0.1),
        "c_out_norm_g": 1.0 + nrm(ks[11], (L, C_HEAD_V), 0.02),
        "d_conv_w": nrm(ks[12], (L, CONV_W, BRANCH_W), CONV_W ** -0.5),
        "d_conv_b": nrm(ks[13], (L, BRANCH_W), 0.02),
        "w_branch_out": nrm(ks[14], (L, N_BRANCH, BRANCH_W, D_MODEL), BRANCH_W ** -0.5),
        "w_merge_gate": nrm(ks[15], (L, N_BRANCH, D_MODEL, D_MODEL), D_MODEL ** -0.5),
        "b_merge_gate": nrm(ks[16], (L, N_BRANCH, D_MODEL), 0.02),
        "w_out": nrm(ks[17], (L, D_MODEL, D_MODEL), D_MODEL ** -0.5),
    }


def reference(x, norm_g, w_in, a_ln_g, a_ln_b, a_spatial_w, a_spatial_b,
              b_q_norm_g, b_k_norm_g, c_gate_w2, c_gate_b, c_out_norm_g,
              d_conv_w, d_conv_b, w_branch_out, w_merge_gate, b_merge_gate, w_out):
    for l in range(DEPTH):
        h = rms_norm(x, norm_g[l])
        wi = w_in[l]
        y_a = spatial_gating_branch(h @ wi[:, A_OFF:B_OFF], a_ln_g[l], a_ln_b[l],
                                    a_spatial_w[l], a_spatial_b[l])
        y_b = moba_branch(h @ wi[:, B_OFF:C_OFF], b_q_norm_g[l], b_k_norm_g[l])
        y_c = gla_branch(h @ wi[:, C_OFF:D_OFF], c_gate_w2[l], c_gate_b[l], c_out_norm_g[l])
        y_d = short_conv_branch(h @ wi[:, D_OFF:N_IN_COLS], d_conv_w[l], d_conv_b[l])
        branches = (y_a, y_b, y_c, y_d)
        merged = jnp.zeros_like(x)
        for i in range(N_BRANCH):
            gate = jax.nn.sigmoid(h @ w_merge_gate[l, i] + b_merge_gate[l, i])
            merged = merged + gate * (branches[i] @ w_branch_out[l, i])
        x = x + merged @ w_out[l]
    return x
```

```python
import math
import numpy as np
import ml_dtypes
import concourse.bass as bass
import concourse.mybir as mybir
from concourse.bass_utils import run_bass_kernel_spmd

F32 = mybir.dt.float32
BF16 = mybir.dt.bfloat16
AF = mybir.ActivationFunctionType
ALU = mybir.AluOpType
AX = mybir.AxisListType

S = 4096
D = 1024
NCOL = 7184
EPS = 1e-6
BIG = 32768.0
TG = 512
NT = TG // 128


class Tr:
    __slots__ = ("w", "r", "ro", "psum")

    def __init__(self, psum=False):
        self.w = None
        self.r = {}
        self.ro = False
        self.psum = psum


class _Eng:
    def __init__(self, name, h, sem):
        self.name = name
        self.h = h
        self.sem = sem
        self.cnt = 0
        self.seen = {}


class Sched:
    def __init__(self, nc, n_dma_slots=24, needed=None):
        self.nc = nc
        self.sems = {}
        self.E = {}
        self.needed = needed
        self.rank = None
        if needed is not None:
            self.rank = {k: {v: i + 1 for i, v in enumerate(vs)} for k, vs in needed.items()}
        self.waited = {}
        for name, h in (("pe", nc.tensor), ("act", nc.scalar), ("dve", nc.vector),
                        ("pool", nc.gpsimd), ("sp", nc.sync)):
            sem = nc.alloc_semaphore("sem_" + name)
            self.sems[name] = sem
            self.E[name] = _Eng(name, h, sem)
        self.slots = {}
        self.slot_i = {}
        for q in ("sp", "pool"):
            self.slots[q] = []
            self.slot_i[q] = 0
            for i in range(n_dma_slots):
                key = "dma_%s%d" % (q, i)
                self.sems[key] = nc.alloc_semaphore("sem_" + key)
                self.slots[q].append([key, 0])
        self.n_ops = 0
        self.n_waits = 0

    def _wait(self, e, deps):
        need = {}
        for d in deps:
            if d is None:
                continue
            k, v = d
            if need.get(k, 0) < v:
                need[k] = v
        for k, v in need.items():
            if e.name == "pe" and k == "pe":
                continue
            if e.seen.get(k, 0) >= v:
                continue
            self.waited.setdefault(k, set()).add(v)
            hv = v
            if self.rank is not None and k in self.rank:
                hv = self.rank[k][v]
            e.h.wait_ge(self.sems[k], hv)
            e.seen[k] = v
            self.n_waits += 1

    @staticmethod
    def _deps(reads, writes, ename=None):
        deps = []
        for t in reads:
            deps.append(t.w)
            if t.psum:
                deps.extend(kv for kv in t.r.items() if kv[0] != ename)
        for t in writes:
            deps.append(t.w)
            deps.extend(t.r.items())
        return deps

    @staticmethod
    def _commit(tok, reads, writes):
        k, v = tok
        for t in reads:
            if not t.ro:
                if t.r.get(k, 0) < v:
                    t.r[k] = v
        for t in writes:
            t.w = tok
            t.r = {}

    def op(self, ename, fn, reads=(), writes=()):
        e = self.E[ename]
        self._wait(e, self._deps(reads, writes, ename))
        ins = fn(e.h)
        e.cnt += 1
        if self.rank is None or e.cnt in self.rank.get(ename, {}):
            ins.then_inc(e.sem, 1)
        self._commit((ename, e.cnt), reads, writes)
        self.n_ops += 1

    def dma(self, qname, out, in_, reads=(), writes=(), **kw):
        e = self.E[qname]
        slot = self.slots[qname][self.slot_i[qname]]
        self.slot_i[qname] = (self.slot_i[qname] + 1) % len(self.slots[qname])
        deps = self._deps(reads, writes)
        if slot[1] > 0:
            deps.append((slot[0], slot[1] * 16))
        self._wait(e, deps)
        ins = e.h.dma_start(out=out, in_=in_, **kw)
        slot[1] += 1
        ins.then_inc(self.sems[slot[0]], 16)
        self._commit((slot[0], slot[1] * 16), reads, writes)
        self.n_ops += 1

    def wait_all(self, ename, trs):
        e = self.E[ename]
        deps = []
        for t in trs:
            deps.append(t.w)
            deps.extend(t.r.items())
        self._wait(e, deps)


class Pool:
    def __init__(self, name, aps):
        self.name = name
        self.items = [(ap, Tr()) for ap in aps]
        self.free_list = list(range(len(aps)))

    def alloc(self):
        assert self.free_list, "pool %s exhausted" % self.name
        return self.free_list.pop(0)

    def free(self, i):
        assert i not in self.free_list
        self.free_list.append(i)

    def ap(self, i):
        return self.items[i][0]

    def tr(self, i):
        return self.items[i][1]


def alibi_slopes():
    return [2.0 ** (-8.0 * (h + 1) / 4) for h in range(4)]


def make_consts():
    p = np.arange(128)
    c = {}
    c["c_ident"] = np.eye(128, dtype=np.float32)
    c["c_uincl"] = (p[:, None] <= p[None, :]).astype(np.float32)
    c["c_lstrict"] = (p[:, None] > p[None, :]).astype(np.float32)
    sl = alibi_slopes()
    al = np.zeros((128, 4 * 32), np.float32)
    for h in range(4):
        for idx in range(32):
            m = idx - 1
            al[:, h * 32 + idx] = sl[h] * (p - 128.0 * m)
    c["c_alibi"] = al
    cmb = np.zeros((128, 8), np.float32)
    for h in range(4):
        for qt in range(2):
            cmb[:, h * 2 + qt] = -sl[h] * (qt * 128.0 + p) - BIG
    c["c_cmb"] = cmb
    es = np.zeros((128, 17, 128), np.float32)
    for j in range(17):
        es[j, j, :] = 1.0
    c["c_esel"] = es.reshape(128, 17 * 128)
    return c


def build(NL=2, NG=8, dbg_group=None, stages="RABCDMO"):
    needed = _build(NL, NG, dbg_group, None, stages)
    return _build(NL, NG, dbg_group, needed, stages)


def _build(NL, NG, dbg_group, needed, stages="RABCDMO"):
    nc = bass.Bass("TRN2", target_bir_lowering=False)
    K = Sched(nc, needed=needed)

    def din(name, shape):
        return nc.dram_tensor(name, list(shape), F32, kind="ExternalInput").ap()

    x_d = din("x", [S, D])
    norm_g = din("norm_g", [2, D])
    w_in = din("w_in", [2, D, NCOL])
    a_ln_g = din("a_ln_g", [2, 512])
    a_ln_b = din("a_ln_b", [2, 512])
    a_wsT = din("a_wsT", [2, 4, 128, 128])
    a_sb = din("a_spatial_b", [2, 4 * 128])
    b_qg = din("b_q_norm_g", [2, 128])
    b_kg = din("b_k_norm_g", [2, 128])
    c_w2 = din("c_gate_w2", [2, 16, 256])
    c_b2 = din("c_gate_b", [2, 256])
    c_og = din("c_out_norm_g", [2, 128])
    d_cw = din("d_conv_w", [2, 3, 512])
    d_cb = din("d_conv_b", [2, 512])
    w_bo = din("w_branch_out", [2, 4, 512, D])
    w_mg = din("w_merge_gate", [2, 4, D, D])
    b_mg = din("b_merge_gate", [2, 4, D])
    w_o = din("w_out", [2, D, D])
    cst = {k: din(k, v.shape) for k, v in make_consts().items()}
    y_d = nc.dram_tensor("y", [S, D], F32, kind="ExternalOutput").ap()
    dbg_d = None
    if dbg_group is not None:
        dbg_d = nc.dram_tensor("dbg", [4, 128, 4 * TG], F32, kind="ExternalOutput").ap()
    ytr = [Tr() for _ in range(S // 128)]

    def sb(name, shape, dt):
        return nc.alloc_sbuf_tensor(name, list(shape), dt)

    kT = sb("kT", [128, 4, S], BF16); kT_t = Tr()
    vc = sb("vc", [128, 32, 4, 129], BF16); vc_t = Tr()
    hT2 = [sb("hT%d" % i, [128, 8, TG], BF16) for i in range(2)]; hT2_t = [Tr(), Tr()]
    cur_h = {"i": 0}
    yT = [sb("yT%d" % i, [128, 4, TG], BF16) for i in range(4)]
    yT_t = [Tr() for _ in range(4)]
    wring = [sb("wr%d" % i, [128, 8, 512], BF16) for i in range(3)]
    Hraw = [sb("H%d" % i, [128, 1024], F32) for i in range(6)]
    Fraw = [sb("F%d" % i, [128, 512], F32) for i in range(8)]
    HP = Pool("H", [t[:, :] for t in Hraw])
    FP = Pool("F", [t[:, :] for t in Fraw])
    PTP = Pool("PT", [sb("pt%d" % i, [128, 256], BF16)[:, :] for i in range(4)])
    psraw = [nc.alloc_psum_tensor("ps%d" % i, [128, 512], F32) for i in range(8)]
    PS = Pool("PS", [t[:, :] for t in psraw])
    for _i in range(8):
        PS.items[_i][1].psum = True

    def Hf(i):
        return HP.ap(i)

    def Hb(i):
        return HP.ap(i).bitcast(BF16)

    def Ff(i):
        return FP.ap(i)

    def Fb(i):
        return FP.ap(i).bitcast(BF16)

    def Pf(i):
        return PS.ap(i)

    def Pb(i):
        return PS.ap(i).bitcast(BF16)

    def v3(ap, a):
        return ap.rearrange("p (a b) -> p a b", a=a)

    ident_bf = sb("ident_bf", [128, 128], BF16); ident_t = Tr()
    uincl_bf = sb("uincl_bf", [128, 128], BF16); uinclb_t = Tr()
    uincl_f = sb("uincl_f", [128, 128], F32); uinclf_t = Tr()
    lstr_bf = sb("lstr_bf", [128, 128], BF16); lstrb_t = Tr()
    ones_f = sb("ones_f", [128, 128], F32); ones_t = Tr()
    alibi = sb("alibi", [128, 128], F32); alibi_t = Tr()
    cmb = sb("cmb", [128, 8], F32); cmb_t = Tr()
    esel = sb("esel", [128, 17, 128], BF16); esel_t = Tr()
    epsc = sb("epsc", [128, 1], F32); epsc_t = Tr()
    gbc = sb("gbc", [128, D], F32); gbc_t = Tr()
    lng = sb("lng", [128, 512], F32); lng_t = Tr()
    lnb = sb("lnb", [128, 512], F32); lnb_t = Tr()
    WsT = sb("WsT", [128, 4, 128], BF16); WsT_t = Tr()
    bsf = sb("bsf", [1, 512], F32); bsf_t = Tr()
    gq_col = sb("gq_col", [128, 1], F32); gq_t = Tr()
    gk_col = sb("gk_col", [128, 1], F32); gk_t = Tr()
    og_col = sb("og_col", [128, 1], F32); og_t = Tr()
    w2b = sb("w2b", [32, 256], BF16); w2b_t = Tr()
    cw = sb("cw", [128, 3, 4], F32); cw_t = Tr()
    cb = sb("cb", [128, 4], F32); cb_t = Tr()
    bmg = sb("bmg", [128, 4, 8], F32); bmg_t = Tr()
    kmT = sb("kmT", [128, 4, 16], BF16); kmT_t = Tr()
    Gs = sb("Gs", [128, 4, 17], F32); Gs_t = Tr()
    msk = sb("msk", [128, 4, 17], F32); msk_t = Tr()
    Aa = sb("Aa", [128, 4, 17], BF16); Aa_t = Tr()
    m8 = sb("m8", [128, 32], F32); m8_t = Tr()
    AT = [sb("AT%d" % i, [128, 4, 256], BF16) for i in range(2)]
    AT_t = [Tr(), Tr()]
    Sst = sb("Sst", [128, 2, 256], F32); Sst_t = Tr()
    Sbf = sb("Sbf", [128, 2, 256], BF16); Sbf_t = Tr()
    uh = sb("uh", [128, 4, 2], F32); uh_t = Tr()
    lrT = sb("lrT", [32, TG], BF16); lrT_t = Tr()
    st16 = sb("st16", [128, 16], F32); st16_t = Tr()
    rs16 = sb("rs16", [128, 16], F32); rs16_t = Tr()
    st16q = sb("st16q", [128, 16], F32); st16q_t = Tr()
    st16k = sb("st16k", [128, 16], F32); st16k_t = Tr()
    rs16q = sb("rs16q", [128, 16], F32); rs16q_t = Tr()
    rs16k = sb("rs16k", [128, 16], F32); rs16k_t = Tr()
    rs16a = sb("rs16a", [128, 4], F32); rs16a_t = Tr()
    st16a = sb("st16a", [128, 4], F32); st16a_t = Tr()
    Aas = [sb("Aas%d" % i, [128, 4, 17], BF16) for i in range(NT)]; Aas_t = [Tr() for _ in range(NT)]
    bnst = sb("bnst", [128, 4, 6], F32); bnst_t = Tr()
    bnmv = sb("bnmv", [128, 4, 2], F32); bnmv_t = Tr()
    kms = sb("kms", [128, 4], F32); kms_t = Tr()
    rc2 = sb("rc2", [128, 2], F32); rc2_t = Tr()
    dec2s = [sb("dec2_%d" % i, [128, 2], F32) for i in range(NT)]; dec2s_t = [Tr() for _ in range(NT)]
    qtls = [sb("qtl%d" % i, [128, 2, 128], BF16) for i in range(NT)]; qtls_t = [Tr() for _ in range(NT)]
    ktls = [sb("ktl%d" % i, [128, 2, 128], BF16) for i in range(NT)]; ktls_t = [Tr() for _ in range(NT)]
    kdecs = [sb("kdec%d" % i, [128, 256], BF16) for i in range(NT)]; kdecs_t = [Tr() for _ in range(NT)]
    attTs = [sb("attT%d" % i, [128, 4, 128], BF16) for i in range(NT)]; attTs_t = [Tr() for _ in range(NT)]

    def mm(out, lhsT, rhs, start, stop, reads, wtr):
        K.op("pe", lambda e: e.matmul(out, lhsT=lhsT, rhs=rhs, start=start, stop=stop),
             reads=reads, writes=[wtr])

    def tp(out, in_, reads, wtr):
        K.op("pe", lambda e: e.transpose(out=out, in_=in_, identity=ident_bf[:, :]),
             reads=list(reads) + [ident_t], writes=[wtr])

    def act(out, in_, func, reads, writes, **kw):
        K.op("act", lambda e: e.activation(out=out, in_=in_, func=func, **kw),
             reads=reads, writes=writes)

    def tt(out, in0, in1, op, reads, writes, eng="dve"):
        K.op(eng, lambda e: e.tensor_tensor(out=out, in0=in0, in1=in1, op=op),
             reads=reads, writes=writes)

    def ts(out, in0, s1, s2, op0, op1, reads, writes, eng="dve"):
        if op1 is None:
            K.op(eng, lambda e: e.tensor_scalar(out=out, in0=in0, scalar1=s1, scalar2=None, op0=op0),
                 reads=reads, writes=writes)
        else:
            K.op(eng, lambda e: e.tensor_scalar(out=out, in0=in0, scalar1=s1, scalar2=s2, op0=op0, op1=op1),
                 reads=reads, writes=writes)

    def stt(out, in0, scalar, in1, op0, op1, reads, writes):
        K.op("dve", lambda e: e.scalar_tensor_tensor(out=out, in0=in0, scalar=scalar, in1=in1, op0=op0, op1=op1),
             reads=reads, writes=writes)

    def rsqrt_to(dst, dst_t, src, src_t, n, scale):
        act(dst[:, 0:n], src[:, 0:n], AF.Ln, [src_t, epsc_t], [dst_t], bias=epsc[:, 0:1], scale=scale)
        act(dst[:, 0:n], dst[:, 0:n], AF.Exp, [dst_t], [dst_t], scale=-0.5)

    def rsqrt_cols(n, scale):
        act(rs16[:, 0:n], st16[:, 0:n], AF.Ln, [st16_t, epsc_t], [rs16_t], bias=epsc[:, 0:1], scale=scale)
        act(rs16[:, 0:n], rs16[:, 0:n], AF.Exp, [rs16_t], [rs16_t], scale=-0.5)

    def wview(ap2d, kk):
        return ap2d.rearrange("(k p) c -> p k c", p=128)

    def layer_blocks(l):
        win = wview(w_in[l], 8)
        out = []

        def blk(key, c0, n):
            out.append((key, [(0, win[:, :, c0:c0 + n], 8, n)], 8, n))
        blk("A_u", 0, 512); blk("A_z", 1024, 512); blk("A_v", 512, 512)
        blk("B_q", 1536, 512); blk("B_k", 2048, 512); blk("B_v", 2560, 512); blk("B_z", 3072, 512)
        blk("C_qk", 3584, 512); blk("C_lr", 4608, 16); blk("C_z", 4624, 512); blk("C_v", 4096, 512)
        for c in range(4):
            parts = []
            for qi, base in enumerate((5648, 6160, 5136, 6672)):
                parts.append((qi * 128, win[:, :, base + c * 128: base + (c + 1) * 128], 8, 128))
            out.append(("D_%d" % c, parts, 8, 512))
        for c2 in range(2):
            for i in range(4):
                out.append(("G_%d_%d" % (i, c2), [(0, wview(w_mg[l, i], 8)[:, :, c2 * 512:(c2 + 1) * 512], 8, 512)], 8, 512))
                out.append(("P_%d_%d" % (i, c2), [(0, wview(w_bo[l, i], 4)[:, :, c2 * 512:(c2 + 1) * 512], 4, 512)], 4, 512))
        for half in range(2):
            out.append(("O_%d" % half, [(0, wview(w_o[l], 8)[:, :, half * 512:(half + 1) * 512], 8, 512)], 8, 512))
        return out

    LB = [layer_blocks(l) for l in range(NL)]
    NB = len(LB[0])
    wsc = nc.dram_tensor("wsc", [NL * NB, 128, 4096], BF16).ap()
    wsc_t = [[Tr() for _ in range(NB)] for _ in range(NL)]
    conv_hist = []
    conv_pos = [0 for _ in range(NL)]

    def wsc_view(l, bi, kk, ntot):
        return wsc[l * NB + bi][:, 0:kk * ntot].rearrange("p (k c) -> p k c", k=kk)

    def convert_blocks(l, count):
        for _ in range(count):
            bi = conv_pos[l]
            if bi >= NB:
                return
            conv_pos[l] += 1
            key, parts, kk, ntot = LB[l][bi]
            dst3 = wsc_view(l, bi, kk, ntot)
            for (c0, src, kk_, n) in parts:
                thr = [conv_hist[-4]] if len(conv_hist) >= 4 else []
                tmp = Tr()
                K.dma("pool", dst3[:, :, c0:c0 + n], src, reads=thr, writes=[wsc_t[l][bi], tmp])
                conv_hist.append(tmp)

    wseq = []
    for l in range(NL):
        for gi in range(NG):
            for bi, (key, parts, kk, ntot) in enumerate(LB[l]):
                wseq.append((key, l, bi, kk, ntot))

    WP = Pool("W", [t[:, :, :] for t in wring])
    wstate = {"issued": 0, "cur": 0, "slot": {}}

    def w_ensure(upto):
        while wstate["issued"] <= upto and wstate["issued"] < len(wseq) and WP.free_list:
            i = wstate["issued"]
            s = WP.alloc()
            key, l_, bi, kk, ntot = wseq[i]
            K.dma("sp", WP.ap(s)[:, 0:kk, 0:ntot], wsc_view(l_, bi, kk, ntot), reads=[wsc_t[l_][bi]], writes=[WP.tr(s)])
            wstate["slot"][i] = s
            wstate["issued"] += 1

    def w_get(key):
        i = wstate["cur"]
        assert wseq[i][0] == key, (wseq[i][0], key)
        w_ensure(i + 2)
        s = wstate["slot"][i]
        return WP.ap(s), WP.tr(s)

    def w_done():
        i = wstate["cur"]
        WP.free(wstate["slot"].pop(i))
        wstate["cur"] += 1
        w_ensure(wstate["cur"] + 2)

    def init_const():
        K.dma("pool", ident_bf[:, :], cst["c_ident"], writes=[ident_t])
        K.dma("pool", uincl_bf[:, :], cst["c_uincl"], writes=[uinclb_t])
        K.dma("sp", uincl_f[:, :], cst["c_uincl"], writes=[uinclf_t])
        K.dma("pool", lstr_bf[:, :], cst["c_lstrict"], writes=[lstrb_t])
        K.dma("sp", alibi[:, :], cst["c_alibi"], writes=[alibi_t])
        K.dma("sp", cmb[:, :], cst["c_cmb"], writes=[cmb_t])
        for j in range(17):
            K.dma("pool", esel[:, j, :], cst["c_esel"][:, j * 128:(j + 1) * 128], writes=[esel_t])
        K.op("dve", lambda e: e.memset(ones_f[:, :], 1.0), writes=[ones_t])
        K.op("dve", lambda e: e.memset(epsc[:, :], EPS), writes=[epsc_t])
        K.op("dve", lambda e: e.memset(lrT[:, :], 1.0), writes=[lrT_t])
        for i_ in range(2):
            K.op("dve", lambda e: e.memset(AT[i_][:, :, :], 0.0), writes=[AT_t[i_]])
        K.op("dve", lambda e: e.memset(vc[:, :, :, 128:129], 1.0), writes=[vc_t])
        for t in (ident_t, uinclb_t, uinclf_t, lstrb_t, alibi_t, cmb_t, esel_t, ones_t, epsc_t):
            t.ro = True

    def init_R(l):
        K.dma("sp", gbc[:, :], norm_g[l:l + 1, :].broadcast_to([128, D]), writes=[gbc_t])

    def init_layer(l):
        K.dma("sp", lng[:, :], a_ln_g[l:l + 1, :].broadcast_to([128, 512]), writes=[lng_t])
        K.dma("sp", lnb[:, :], a_ln_b[l:l + 1, :].broadcast_to([128, 512]), writes=[lnb_t])
        K.dma("sp", bsf[:, :], a_sb[l:l + 1, :], writes=[bsf_t])
        f = FP.alloc()
        for g in range(4):
            K.dma("sp", Ff(f)[:, g * 128:(g + 1) * 128], a_wsT[l, g], writes=[FP.tr(f)])
        tt(WsT[:, :, :], v3(Ff(f), 4), uincl_f[:, :].unsqueeze(1).broadcast_to([128, 4, 128]), ALU.mult,
           [FP.tr(f), uinclf_t], [WsT_t])
        FP.free(f)
        with nc.allow_non_contiguous_dma(reason="tiny per-layer parameter columns"):
            K.dma("sp", gq_col[:, :], b_qg[l:l + 1, :].rearrange("o d -> d o"), writes=[gq_t])
            K.dma("sp", gk_col[:, :], b_kg[l:l + 1, :].rearrange("o d -> d o"), writes=[gk_t])
            K.dma("sp", og_col[:, :], c_og[l:l + 1, :].rearrange("o d -> d o"), writes=[og_t])
            for j in range(3):
                K.dma("sp", cw[:, j, :], d_cw[l, j:j + 1, :].rearrange("o (c p) -> p (o c)", p=128), writes=[cw_t])
            K.dma("sp", cb[:, :], d_cb[l:l + 1, :].rearrange("o (c p) -> p (o c)", p=128), writes=[cb_t])
            for i in range(4):
                K.dma("sp", bmg[:, i, :], b_mg[l, i:i + 1, :].rearrange("o (f p) -> p (o f)", p=128), writes=[bmg_t])
        ts(gq_col[:, :], gq_col[:, :], 128.0 ** -0.5, None, ALU.mult, None, [gq_t], [gq_t])
        K.dma("pool", w2b[0:16, :], c_w2[l], writes=[w2b_t])
        K.dma("pool", w2b[16:17, :], c_b2[l:l + 1, :], writes=[w2b_t])
        K.op("dve", lambda e: e.memset(Sst[:, :, :], 0.0), writes=[Sst_t])
        K.op("dve", lambda e: e.memset(Sbf[:, :, :], 0.0), writes=[Sbf_t])
        K.op("dve", lambda e: e.memset(uh[:, :, :], 0.0), writes=[uh_t])
        K.op("dve", lambda e: e.memset(kmT[:, :, :], 0.0), writes=[kmT_t])
        K.op("dve", lambda e: e.memset(Gs[:, :, 0:16], -1.0e30), writes=[Gs_t])
        K.op("dve", lambda e: e.memset(Gs[:, :, 16:17], 1.0e30), writes=[Gs_t])

    def proj_fm(wb, wtr, c0, evac, nparts=128, kk=8, rhs_of=None, rhs_tr=None):
        p = PS.alloc()
        for k in range(kk):
            rhs = hT2[cur_h["i"]][:, k, :] if rhs_of is None else rhs_of(k)
            mm(Pf(p)[0:nparts, 0:TG], wb[:, k, c0:c0 + nparts], rhs, k == 0, k == kk - 1,
               [wtr, hT2_t[cur_h["i"]] if rhs_tr is None else rhs_tr], PS.tr(p))
        evac(p)
        PS.free(p)

    def proj_tm(wb, wtr, ti, c0, n, evac):
        p = PS.alloc()
        for k in range(8):
            mm(Pf(p)[:, 0:n], hT2[cur_h["i"]][:, k, ti * 128:(ti + 1) * 128], wb[:, k, c0:c0 + n], k == 0, k == 7,
               [wtr, hT2_t[cur_h["i"]]], PS.tr(p))
        evac(p)
        PS.free(p)

    def stage_R(l, gi):
        src = x_d if l == 0 else y_d
        xs = []
        for ti in range(NT):
            T = gi * NT + ti
            h = HP.alloc()
            xs.append(h)
            K.dma("pool", Hf(h), src[T * 128:(T + 1) * 128, :], reads=([ytr[T]] if l > 0 else []), writes=[HP.tr(h)])
            f = FP.alloc()
            act(Fb(f)[:, 0:D], Hf(h), AF.Square, [HP.tr(h)], [FP.tr(f), st16_t], accum_out=st16[:, ti:ti + 1])
            FP.free(f)
        rsqrt_cols(NT, 1.0 / D)
        hbuf = hT2[cur_h["i"]]
        hbuf_t = hT2_t[cur_h["i"]]
        fs = []
        for ti in range(NT):
            h = xs[ti]
            f = FP.alloc()
            stt(Fb(f)[:, 0:D], Hf(h), rs16[:, ti:ti + 1], gbc[:, :], ALU.mult, ALU.mult,
                [HP.tr(h), rs16_t, gbc_t], [FP.tr(f)])
            HP.free(h)
            fs.append(f)

        def pe_part():
            for ti in range(NT):
                f = fs[ti]
                p = PS.alloc()
                for c in range(8):
                    tp(Pb(p)[:, c * 128:(c + 1) * 128], Fb(f)[:, c * 128:(c + 1) * 128], [FP.tr(f)], PS.tr(p))
                K.op("act", lambda e: e.copy(out=hbuf[:, :, ti * 128:(ti + 1) * 128], in_=v3(Pb(p), 8)),
                     reads=[PS.tr(p)], writes=[hbuf_t])
                PS.free(p)
                FP.free(f)
        return pe_part

    def stage_A1(l, gi):
        wb, wtr = w_get("A_u")
        gu = HP.alloc()
        for c in range(4):
            proj_fm(wb, wtr, c * 128, lambda p: act(v3(Hb(gu), 4)[:, c, :], Pf(p)[:, 0:TG], AF.Gelu_apprx_tanh,
                                                    [PS.tr(p)], [HP.tr(gu)]))
        w_done()
        wb, wtr = w_get("A_z")
        sz = HP.alloc()
        for c in range(4):
            proj_fm(wb, wtr, c * 128, lambda p: act(v3(Hb(sz), 4)[:, c, :], Pf(p)[:, 0:TG], AF.Silu,
                                                    [PS.tr(p)], [HP.tr(sz)]))
        w_done()
        tt(Hb(gu), Hb(gu), Hb(sz), ALU.mult, [HP.tr(gu), HP.tr(sz)], [HP.tr(gu)])
        HP.free(sz)
        wb, wtr = w_get("A_v")
        gv = []
        for ti in range(NT):
            f = FP.alloc()
            gv.append(f)
            proj_tm(wb, wtr, ti, 0, 512, lambda p: act(Ff(f), Pf(p), AF.Gelu_apprx_tanh, [PS.tr(p)], [FP.tr(f)]))
            K.op("dve", lambda e: e.bn_stats(out=bnst[:, ti, :], in_=Ff(f)), reads=[FP.tr(f)], writes=[bnst_t])
            K.op("dve", lambda e: e.bn_aggr(out=bnmv[:, ti, :], in_=bnst[:, ti, :]), reads=[bnst_t], writes=[bnmv_t])
        w_done()
        K.op("dve", lambda e: e.tensor_copy(out=st16a[:, 0:NT], in_=bnmv[:, :, 1]), reads=[bnmv_t], writes=[st16a_t])
        rsqrt_to(rs16a, rs16a_t, st16a, st16a_t, NT, 1.0)
        vln = []
        for ti in range(NT):
            f = gv[ti]
            ts(Ff(f), Ff(f), bnmv[:, ti, 0:1], rs16a[:, ti:ti + 1], ALU.subtract, ALU.mult,
               [FP.tr(f), bnmv_t, rs16a_t], [FP.tr(f)])
            tt(Ff(f), Ff(f), lng[:, :], ALU.mult, [FP.tr(f), lng_t], [FP.tr(f)])
            f2 = FP.alloc()
            tt(Fb(f2)[:, 0:512], Ff(f), lnb[:, :], ALU.add, [FP.tr(f), lnb_t], [FP.tr(f2)])
            FP.free(f)
            vln.append(f2)
        return gu, vln

    def stage_A2(l, gi, gu, vln):
        for ti in range(NT):
            f2 = vln[ti]
            p = PS.alloc()
            for g in range(4):
                mm(Pf(p)[:, g * 128:(g + 1) * 128], Fb(f2)[:, g * 128:(g + 1) * 128], WsT[:, g, :], True, False,
                   [FP.tr(f2), WsT_t], PS.tr(p))
                mm(Pf(p)[:, g * 128:(g + 1) * 128], ones_f[0:1, 0:128], bsf[0:1, g * 128:(g + 1) * 128], False, True,
                   [ones_t, bsf_t], PS.tr(p))
            tt(yT[0][:, :, ti * 128:(ti + 1) * 128], v3(Pf(p), 4), v3(Hb(gu), 4)[:, :, ti * 128:(ti + 1) * 128], ALU.mult,
               [PS.tr(p), HP.tr(gu)], [yT_t[0]])
            PS.free(p)
            FP.free(f2)
        HP.free(gu)

    def stage_B1(l, gi):
        raws = {}
        for key, ssq, ssq_t in (("B_q", st16q, st16q_t), ("B_k", st16k, st16k_t)):
            wb, wtr = w_get(key)
            hs = [HP.alloc(), HP.alloc()]
            raws[key] = hs
            for ti in range(NT):
                dst = v3(Hf(hs[ti // 2]), 2)[:, ti % 2, :]
                dtr = HP.tr(hs[ti // 2])
                proj_tm(wb, wtr, ti, 0, 512, lambda p: K.op("act", lambda e: e.copy(out=dst, in_=Pf(p)),
                                                             reads=[PS.tr(p)], writes=[dtr]))
                f2 = FP.alloc()
                tt(Ff(f2), dst, dst, ALU.mult, [dtr], [FP.tr(f2)])
                K.op("dve", lambda e: e.tensor_reduce(out=ssq[:, ti * 4:(ti + 1) * 4], in_=v3(Ff(f2), 4), axis=AX.X, op=ALU.add),
                     reads=[FP.tr(f2)], writes=[ssq_t])
                FP.free(f2)
            w_done()
        return raws

    def stage_B2(l, gi, raws):
        qT = HP.alloc()
        qTv = v3(Hb(qT), 4)
        rsqrt_to(rs16q, rs16q_t, st16q, st16q_t, 16, 1.0 / 128)
        rsqrt_to(rs16k, rs16k_t, st16k, st16k_t, 16, 1.0 / 128)
        wb, wtr = w_get("B_v")
        for ti in range(NT):
            T = gi * NT + ti
            proj_tm(wb, wtr, ti, 0, 512, lambda p: K.op("act", lambda e: e.copy(out=vc[:, T, :, 0:128], in_=v3(Pf(p), 4)),
                                                         reads=[PS.tr(p)], writes=[vc_t]))
        w_done()
        for key, ssq, ssq_t, rsx, rsx_t, gcol, gtr, dst_fn, dst_tr in (
                ("B_q", st16q, st16q_t, rs16q, rs16q_t, gq_col, gq_t,
                 lambda ti: qTv[:, :, ti * 128:(ti + 1) * 128], HP.tr(qT)),
                ("B_k", st16k, st16k_t, rs16k, rs16k_t, gk_col, gk_t,
                 lambda ti: kT[:, :, (gi * NT + ti) * 128:(gi * NT + ti + 1) * 128], kT_t)):
            hs = raws[key]
            for ti in range(NT):
                src = v3(Hf(hs[ti // 2]), 2)[:, ti % 2, :]
                f2 = FP.alloc()
                tt(v3(Fb(f2)[:, 0:512], 4), v3(src, 4), rsx[:, ti * 4:(ti + 1) * 4].unsqueeze(2).broadcast_to([128, 4, 128]),
                   ALU.mult, [HP.tr(hs[ti // 2]), rsx_t], [FP.tr(f2)])
                p = PS.alloc()
                for h in range(4):
                    tp(Pb(p)[:, h * 128:(h + 1) * 128], Fb(f2)[:, h * 128:(h + 1) * 128], [FP.tr(f2)], PS.tr(p))
                act(dst_fn(ti), v3(Pb(p)[:, 0:512], 4), AF.Identity, [PS.tr(p), gtr], [dst_tr], scale=gcol[:, 0:1])
                PS.free(p)
                FP.free(f2)
            HP.free(hs[0])
            HP.free(hs[1])
        for bl in range(2):
            B = gi * 2 + bl
            K.op("dve", lambda e: e.tensor_reduce(out=kms[:, :], in_=kT[:, :, B * 256:(B + 1) * 256], axis=AX.X, op=ALU.add),
                 reads=[kT_t], writes=[kms_t])
            ts(kmT[:, :, B], kms[:, :], 1.0 / 256, None, ALU.mult, None, [kms_t], [kmT_t])
        def topk_tile(ti):
            T = gi * NT + ti
            B = T // 2
            par = T % 2
            if B > 0:
                p = PS.alloc()
                for h in range(4):
                    mm(Pf(p)[:, h * 16:(h + 1) * 16], qTv[:, h, ti * 128:(ti + 1) * 128], kmT[:, h, 0:16], True, True,
                       [HP.tr(qT), kmT_t], PS.tr(p))
                K.op("dve", lambda e: e.tensor_copy(out=Gs[:, :, 0:B], in_=v3(Pf(p)[:, 0:64], 4)[:, :, 0:B]),
                     reads=[PS.tr(p)], writes=[Gs_t])
                PS.free(p)
            for h in range(4):
                K.op("dve", lambda e: e.max(out=m8[:, h * 8:(h + 1) * 8], in_=Gs[:, h, 0:16]), reads=[Gs_t], writes=[m8_t])
            for h in range(4):
                ts(msk[:, h, :], Gs[:, h, :], m8[:, h * 8 + 2:h * 8 + 3], None, ALU.is_ge, None, [Gs_t, m8_t], [msk_t])
            for h in range(4):
                ts(Aas[ti][:, h, :], msk[:, h, :], BIG, cmb[:, h * 2 + par:h * 2 + par + 1], ALU.mult, ALU.add,
                   [msk_t, cmb_t], [Aas_t[ti]])

        def at_tile(ti):
            T = gi * NT + ti
            B = T // 2
            par = T % 2
            at = AT[B % 2]
            at_t = AT_t[B % 2]
            p = PS.alloc()
            for h in range(4):
                tp(Pb(p)[0:17, h * 128:(h + 1) * 128], Aas[ti][:, h, :], [Aas_t[ti]], PS.tr(p))
            K.op("act", lambda e: e.copy(out=at[0:17, :, par * 128:(par + 1) * 128], in_=v3(Pb(p)[0:17, 0:512], 4)),
                 reads=[PS.tr(p)], writes=[at_t])
            PS.free(p)

        topk_tile(0)
        topk_tile(1)
        wb, wtr = w_get("B_z")
        szb = HP.alloc()
        for c in range(4):
            proj_fm(wb, wtr, c * 128, lambda p: act(v3(Hb(szb), 4)[:, c, :], Pf(p)[:, 0:TG], AF.Silu,
                                                    [PS.tr(p)], [HP.tr(szb)]))
        w_done()
        at_tile(0)
        at_tile(1)
        topk_tile(2)
        topk_tile(3)
        steps = []
        for bl in range(2):
            for h in range(4):
                for kt in range(2 * (gi * 2 + bl) + 2):
                    steps.append((bl, h, kt))
        LA = 2
        obs = [FP.alloc(), FP.alloc()]
        obvs = [Fb(o).rearrange("p (q h d) -> p q h d", q=2, h=4) for o in obs]
        issued = {}
        accs = {}

        first_b1 = 4 * (2 * (gi * 2) + 2)

        def issue(i):
            if i == first_b1:
                at_tile(2)
                at_tile(3)
            bl, h, kt = steps[i]
            B = gi * 2 + bl
            at, at_t = AT[B % 2], AT_t[B % 2]
            qloc = bl * 256
            j = kt // 2
            qs_ = 0 if kt <= 2 * B else 128
            n = 256 - qs_
            pss = PS.alloc()
            mm(Pf(pss)[:, 0:n], kT[:, h, kt * 128:(kt + 1) * 128], qTv[:, h, qloc + qs_:qloc + 256], True, False,
               [kT_t, HP.tr(qT)], PS.tr(pss))
            jsel = j if j < B else 16
            mm(Pf(pss)[:, 0:n], esel[:, jsel, :], at[:, h, qs_:256], False, True, [esel_t, at_t], PS.tr(pss))
            issued[i] = (pss, n, qs_)

        def finish_block(bl):
            for qt in range(2):
                tcol = (bl * 2 + qt) * 128
                p = PS.alloc()
                for h in range(4):
                    tp(Pb(p)[:, h * 128:(h + 1) * 128], obvs[bl][:, qt, h, :], [FP.tr(obs[bl])], PS.tr(p))
                tt(yT[1][:, :, tcol:tcol + 128], v3(Pb(p)[:, 0:512], 4), v3(Hb(szb), 4)[:, :, tcol:tcol + 128], ALU.mult,
                   [PS.tr(p), HP.tr(szb)], [yT_t[1]])
                PS.free(p)
            FP.free(obs[bl])

        deferred = []
        for i in range(min(LA, len(steps))):
            issue(i)
        for i, (bl, h, kt) in enumerate(steps):
            if i + LA < len(steps):
                issue(i + LA)
            B = gi * 2 + bl
            nkt = 2 * B + 2
            if kt == 0:
                accs[(bl, h)] = [PS.alloc(), PS.alloc()]
            ac = accs[(bl, h)]
            pss, n, qs_ = issued.pop(i)
            pt = PTP.alloc()
            aidx = h * 32 + (2 * B - kt + 1)
            act(PTP.ap(pt)[:, 0:n], Pf(pss)[:, 0:n], AF.Exp, [PS.tr(pss), alibi_t], [PTP.tr(pt)],
                bias=alibi[:, aidx:aidx + 1], scale=1.0)
            PS.free(pss)
            if kt >= 2 * B:
                tt(PTP.ap(pt)[:, 0:128], PTP.ap(pt)[:, 0:128], uincl_bf[:, :], ALU.mult,
                   [PTP.tr(pt), uinclb_t], [PTP.tr(pt)])
            for qt in range(2):
                if kt <= 2 * B + qt:
                    c0 = qt * 128 - qs_
                    mm(Pf(ac[qt])[:, 0:129], PTP.ap(pt)[:, c0:c0 + 128], vc[:, kt, h, :], kt == 0, kt == 2 * B + qt,
                       [PTP.tr(pt), vc_t], PS.tr(ac[qt]))
            PTP.free(pt)
            if kt == nkt - 1:
                for qt in range(2):
                    a_ = ac[qt]
                    K.op("dve", lambda e: e.reciprocal(out=rc2[:, qt:qt + 1], in_=Pf(a_)[:, 128:129]),
                         reads=[PS.tr(a_)], writes=[rc2_t])
                    ts(obvs[bl][:, qt, h, :], Pf(a_)[:, 0:128], rc2[:, qt:qt + 1], None, ALU.mult, None,
                       [PS.tr(a_), rc2_t], [FP.tr(obs[bl])])
                    PS.free(a_)
                del accs[(bl, h)]
                if h == 3:
                    deferred.append((i + LA + 2, bl))
            while deferred and deferred[0][0] <= i:
                finish_block(deferred.pop(0)[1])

        def tail():
            while deferred:
                finish_block(deferred.pop(0)[1])
            HP.free(qT)
            HP.free(szb)
        return tail

    def stage_C(l, gi, dgen=None, pre=None):
        wb, wtr = w_get("C_qk")
        qf = HP.alloc()
        kf = HP.alloc()
        ktm = HP.alloc()
        for c in range(4):
            dst = v3(Hf(qf), 2)[:, c, :] if c < 2 else v3(Hf(kf), 2)[:, c - 2, :]
            dtr = HP.tr(qf) if c < 2 else HP.tr(kf)
            proj_fm(wb, wtr, c * 128, lambda p: K.op("act", lambda e: e.copy(out=dst, in_=Pf(p)[:, 0:TG]),
                                                     reads=[PS.tr(p)], writes=[dtr]))
        for ti in range(NT):
            proj_tm(wb, wtr, ti, 256, 256, lambda p: K.op("act", lambda e: e.copy(out=v3(Hf(ktm), 4)[:, ti, :], in_=Pf(p)[:, 0:256]),
                                                           reads=[PS.tr(p)], writes=[HP.tr(ktm)]))
        w_done()
        if pre is not None:
            pre()
        wb, wtr = w_get("C_lr")
        proj_fm(wb, wtr, 0, lambda p: K.op("act", lambda e: e.copy(out=lrT[0:16, :], in_=Pf(p)[0:16, 0:TG]),
                                           reads=[PS.tr(p)], writes=[lrT_t]), nparts=16)
        w_done()
        wb, wtr = w_get("C_z")
        szc = HP.alloc()
        for c in range(4):
            proj_fm(wb, wtr, c * 128, lambda p: act(v3(Hb(szc), 4)[:, c, :], Pf(p)[:, 0:TG], AF.Silu,
                                                    [PS.tr(p)], [HP.tr(szc)]))
        w_done()
        wb, wtr = w_get("C_v")
        vg = HP.alloc()
        for ti in range(NT):
            proj_tm(wb, wtr, ti, 0, 512, lambda p: K.op("act", lambda e: e.copy(out=v3(Hb(vg), 4)[:, ti, :], in_=Pf(p)),
                                                         reads=[PS.tr(p)], writes=[HP.tr(vg)]))
        w_done()
        vgv = v3(Hb(vg), 4)
        hls = []
        for ti in range(NT):
            tsl = slice(ti * 128, (ti + 1) * 128)
            p = PS.alloc()
            mm(Pf(p)[:, 0:256], lrT[0:17, tsl], w2b[0:17, :], True, True, [lrT_t, w2b_t], PS.tr(p))
            lg = FP.alloc()
            act(Ff(lg)[:, 0:256], Pf(p)[:, 0:256], AF.Exp, [PS.tr(p)], [FP.tr(lg)], scale=-1.0)
            PS.free(p)
            act(Ff(lg)[:, 0:256], Ff(lg)[:, 0:256], AF.Ln, [FP.tr(lg)], [FP.tr(lg)], bias=1.0, scale=1.0)
            hl = FP.alloc()
            hlv = Fb(hl)
            K.op("dve", lambda e: e.tensor_copy(out=hlv[:, 0:256], in_=Ff(lg)[:, 0:256]), reads=[FP.tr(lg)], writes=[FP.tr(hl)])
            tt(hlv[:, 256:512], Ff(lg)[:, 0:256], hlv[:, 0:256], ALU.subtract, [FP.tr(lg), FP.tr(hl)], [FP.tr(hl)])
            FP.free(lg)
            hls.append(hl)
        for ti in range(NT):
            tsl = slice(ti * 128, (ti + 1) * 128)
            dec2, dec2_t = dec2s[ti], dec2s_t[ti]
            qtl, qtl_t = qtls[ti], qtls_t[ti]
            ktl, ktl_t = ktls[ti], ktls_t[ti]
            kdec, kdec_t = kdecs[ti], kdecs_t[ti]
            hl = hls[ti]
            hlv = Fb(hl)
            p = PS.alloc()
            for c in range(2):
                mm(Pf(p)[:, c * 128:(c + 1) * 128], hlv[:, c * 128:(c + 1) * 128], uincl_bf[:, :], True, False,
                   [FP.tr(hl), uinclb_t], PS.tr(p))
                mm(Pf(p)[:, c * 128:(c + 1) * 128], hlv[:, 256 + c * 128:256 + (c + 1) * 128], uincl_bf[:, :], False, True,
                   [FP.tr(hl), uinclb_t], PS.tr(p))
            mm(Pf(p)[:, 256:512], lstr_bf[:, :], hlv[:, 0:256], True, False, [FP.tr(hl), lstrb_t], PS.tr(p))
            mm(Pf(p)[:, 256:512], lstr_bf[:, :], hlv[:, 256:512], False, True, [FP.tr(hl), lstrb_t], PS.tr(p))
            FP.free(hl)
            e1 = FP.alloc()
            e2 = FP.alloc()
            act(Ff(e1)[:, 0:256], Pf(p)[:, 0:256], AF.Exp, [PS.tr(p)], [FP.tr(e1)], scale=-1.0 / 16)
            act(Ff(e1)[:, 256:512], Pf(p)[:, 0:256], AF.Exp, [PS.tr(p)], [FP.tr(e1)], scale=1.0 / 16)
            act(Ff(e2)[:, 0:256], Pf(p)[:, 256:512], AF.Exp, [PS.tr(p)], [FP.tr(e2)], scale=-1.0 / 16)
            act(dec2[:, :], v3(Pf(p)[:, 0:256], 2)[:, :, 127], AF.Exp, [PS.tr(p)], [dec2_t], scale=-1.0 / 16)
            PS.free(p)
            stt(qtl[:, :, :], v3(Hf(qf), 2)[:, :, tsl], 0.125, v3(Ff(e1)[:, 0:256], 2), ALU.mult, ALU.mult,
                [HP.tr(qf), FP.tr(e1)], [qtl_t])
            tt(ktl[:, :, :], v3(Hf(kf), 2)[:, :, tsl], v3(Ff(e1)[:, 256:512], 2), ALU.mult,
               [HP.tr(kf), FP.tr(e1)], [ktl_t])
            tt(kdec[:, :], v3(Hf(ktm), 4)[:, ti, :], Ff(e2)[:, 0:256], ALU.mult, [HP.tr(ktm), FP.tr(e2)], [kdec_t])
            FP.free(e1)
            FP.free(e2)
        for ti in range(NT):
            qtl, qtl_t = qtls[ti], qtls_t[ti]
            ktl, ktl_t = ktls[ti], ktls_t[ti]
            attT, attT_t = attTs[ti], attTs_t[ti]
            pa = [PS.alloc(), PS.alloc()]
            for h in range(4):
                c, po = h // 2, 64 * (h % 2)
                mm(Pf(pa[h % 2])[:, c * 128:(c + 1) * 128], ktl[po:po + 64, c, :], qtl[po:po + 64, c, :], True, True,
                   [ktl_t, qtl_t], PS.tr(pa[h % 2]))
            for r in range(2):
                tt(attT[:, r::2, :], v3(Pf(pa[r])[:, 0:256], 2), uincl_f[:, :].unsqueeze(1).broadcast_to([128, 2, 128]), ALU.mult,
                   [PS.tr(pa[r]), uinclf_t], [attT_t])
                PS.free(pa[r])

        def finish_tile(ti, sq):
            tsl = slice(ti * 128, (ti + 1) * 128)
            p = PS.alloc()
            for h in range(4):
                tp(Pb(p)[:, h * 128:(h + 1) * 128], Fb(sq)[:, h * 128:(h + 1) * 128], [FP.tr(sq)], PS.tr(p))
            stt(yT[2][:, :, tsl], v3(Pb(p)[:, 0:512], 4), og_col[:, 0:1], v3(Hb(szc), 4)[:, :, tsl], ALU.mult, ALU.mult,
                [PS.tr(p), og_t, HP.tr(szc)], [yT_t[2]])
            PS.free(p)
            FP.free(sq)

        pend = None
        for ti in range(NT):
            dec2, dec2_t = dec2s[ti], dec2s_t[ti]
            qtl, qtl_t = qtls[ti], qtls_t[ti]
            kdec, kdec_t = kdecs[ti], kdecs_t[ti]
            attT, attT_t = attTs[ti], attTs_t[ti]
            po_ = PS.alloc()
            for h in range(4):
                c, po = h // 2, 64 * (h % 2)
                mm(Pf(po_)[:, h * 128:(h + 1) * 128], attT[:, h, :], vgv[:, ti, h * 128:(h + 1) * 128], True, False,
                   [attT_t, HP.tr(vg)], PS.tr(po_))
                mm(Pf(po_)[:, h * 128:(h + 1) * 128], qtl[po:po + 64, c, :], Sbf[po:po + 64, c, (h % 2) * 128:(h % 2 + 1) * 128],
                   False, True, [qtl_t, Sbf_t], PS.tr(po_))
            p = PS.alloc()
            for c in range(2):
                mm(Pf(p)[:, c * 256:(c + 1) * 256], kdec[:, c * 128:(c + 1) * 128], vgv[:, ti, c * 256:(c + 1) * 256], True, True,
                   [kdec_t, HP.tr(vg)], PS.tr(p))
            for c in range(2):
                stt(Sst[:, c, :], Sst[:, c, :], dec2[:, c:c + 1], Pf(p)[:, c * 256:(c + 1) * 256], ALU.mult, ALU.add,
                    [Sst_t, dec2_t, PS.tr(p)], [Sst_t])
            PS.free(p)
            K.op("act", lambda e: e.copy(out=Sbf[:, :, :], in_=Sst[:, :, :]), reads=[Sst_t], writes=[Sbf_t])
            if dgen is not None:
                next(dgen, None)
            if pend is not None:
                finish_tile(*pend)
            osb = FP.alloc()
            K.op("act", lambda e: e.copy(out=Ff(osb), in_=Pf(po_)), reads=[PS.tr(po_)], writes=[FP.tr(osb)])
            PS.free(po_)
            sq = FP.alloc()
            tt(Ff(sq), Ff(osb), Ff(osb), ALU.mult, [FP.tr(osb)], [FP.tr(sq)])
            K.op("dve", lambda e: e.tensor_reduce(out=st16[:, 0:4], in_=v3(Ff(sq), 4), axis=AX.X, op=ALU.add),
                 reads=[FP.tr(sq)], writes=[st16_t])
            rsqrt_cols(4, 1.0 / 128)
            tt(v3(Fb(sq)[:, 0:512], 4), v3(Ff(osb), 4), rs16[:, 0:4].unsqueeze(2).broadcast_to([128, 4, 128]), ALU.mult,
               [FP.tr(osb), rs16_t], [FP.tr(sq)])
            FP.free(osb)
            pend = (ti, sq)
        for h_ in (qf, kf, ktm, vg):
            HP.free(h_)

        def ctail():
            finish_tile(*pend)
            HP.free(szc)
        return ctail

    def stage_D(l, gi):
        for c in range(4):
            wb, wtr = w_get("D_%d" % c)
            ps4 = []
            for qi in range(4):
                p = PS.alloc()
                for k in range(8):
                    mm(Pf(p)[:, 0:TG], wb[:, k, qi * 128:(qi + 1) * 128], hT2[cur_h["i"]][:, k, :], k == 0, k == 7, [wtr, hT2_t[cur_h["i"]]], PS.tr(p))
                ps4.append(p)
            w_done()
            p_cg, p_xin, p_bg, p_z = ps4
            u = FP.alloc()
            K.op("act", lambda e: e.copy(out=Ff(u), in_=Pf(p_cg)), reads=[PS.tr(p_cg)], writes=[FP.tr(u)])
            PS.free(p_cg)
            tt(Ff(u), Ff(u), Pf(p_xin), ALU.mult, [FP.tr(u), PS.tr(p_xin)], [FP.tr(u)])
            PS.free(p_xin)
            y = FP.alloc()
            ts(Ff(y), Ff(u), cw[:, 2, c:c + 1], cb[:, c:c + 1], ALU.mult, ALU.add, [FP.tr(u), cw_t, cb_t], [FP.tr(y)])
            stt(Ff(y)[:, 1:512], Ff(u)[:, 0:511], cw[:, 1, c:c + 1], Ff(y)[:, 1:512], ALU.mult, ALU.add,
                [FP.tr(u), cw_t, FP.tr(y)], [FP.tr(y)])
            stt(Ff(y)[:, 2:512], Ff(u)[:, 0:510], cw[:, 0, c:c + 1], Ff(y)[:, 2:512], ALU.mult, ALU.add,
                [FP.tr(u), cw_t, FP.tr(y)], [FP.tr(y)])
            stt(Ff(y)[:, 0:1], uh[:, c, 1:2], cw[:, 1, c:c + 1], Ff(y)[:, 0:1], ALU.mult, ALU.add,
                [uh_t, cw_t, FP.tr(y)], [FP.tr(y)])
            stt(Ff(y)[:, 0:2], uh[:, c, 0:2], cw[:, 0, c:c + 1], Ff(y)[:, 0:2], ALU.mult, ALU.add,
                [uh_t, cw_t, FP.tr(y)], [FP.tr(y)])
            K.op("dve", lambda e: e.tensor_copy(out=uh[:, c, :], in_=Ff(u)[:, 510:512]), reads=[FP.tr(u)], writes=[uh_t])
            FP.free(u)
            sz = FP.alloc()
            act(Ff(sz), Pf(p_z), AF.Silu, [PS.tr(p_z)], [FP.tr(sz)])
            PS.free(p_z)
            tt(Ff(y), Ff(y), Ff(sz), ALU.mult, [FP.tr(y), FP.tr(sz)], [FP.tr(y)])
            FP.free(sz)
            tt(yT[3][:, c, :], Ff(y), Pf(p_bg), ALU.mult, [FP.tr(y), PS.tr(p_bg)], [yT_t[3]])
            PS.free(p_bg)
            FP.free(y)
            yield c

    def stage_M(l, gi, mid=None):
        mT = [HP.alloc(), HP.alloc()]
        for c2 in range(2):
            ma = [HP.alloc(), HP.alloc()]

            def macc(f):
                return v3(Hf(ma[f // 2]), 2)[:, f % 2, :], HP.tr(ma[f // 2])
            for i in range(4):
                wb, wtr = w_get("G_%d_%d" % (i, c2))
                gf = [FP.alloc(), FP.alloc()]
                gh = HP.alloc()
                gaps = [Ff(gf[0]), Ff(gf[1]), v3(Hf(gh), 2)[:, 0, :], v3(Hf(gh), 2)[:, 1, :]]
                gtrs = [FP.tr(gf[0]), FP.tr(gf[1]), HP.tr(gh), HP.tr(gh)]
                for f in range(4):
                    fidx = c2 * 4 + f
                    proj_fm(wb, wtr, f * 128, lambda p: act(gaps[f], Pf(p)[:, 0:TG], AF.Sigmoid, [PS.tr(p), bmg_t], [gtrs[f]],
                                                            bias=bmg[:, i, fidx:fidx + 1], scale=1.0))
                w_done()
                wb, wtr = w_get("P_%d_%d" % (i, c2))
                for f in range(4):
                    gap, gtr_ = gaps[f], gtrs[f]
                    mac, mtr = macc(f)

                    def ev(p):
                        if i == 0:
                            tt(mac, gap, Pf(p)[:, 0:TG], ALU.mult, [gtr_, PS.tr(p)], [mtr])
                        else:
                            tt(gap, gap, Pf(p)[:, 0:TG], ALU.mult, [gtr_, PS.tr(p)], [gtr_])
                            if i < 3:
                                tt(mac, mac, gap, ALU.add, [mtr, gtr_], [mtr])
                            else:
                                tt(v3(Hb(mT[c2]), 4)[:, f, :], mac, gap, ALU.add, [mtr, gtr_], [HP.tr(mT[c2])])
                    proj_fm(wb, wtr, f * 128, ev, kk=4, rhs_of=lambda k: yT[i][:, k, :], rhs_tr=yT_t[i])
                FP.free(gf[0])
                FP.free(gf[1])
                HP.free(gh)
                w_done()
                if mid is not None and c2 == 0 and i == 1:
                    mid()
            HP.free(ma[0])
            HP.free(ma[1])
        return mT

    def stage_O(l, gi, mT):
        for half in range(2):
            wb, wtr = w_get("O_%d" % half)
            src = x_d if l == 0 else y_d
            for ti in range(NT):
                T = gi * NT + ti
                xs = FP.alloc()
                K.dma("pool", Ff(xs), src[T * 128:(T + 1) * 128, half * 512:(half + 1) * 512],
                      reads=([ytr[T]] if l > 0 else []), writes=[FP.tr(xs)])
                p = PS.alloc()
                for f in range(8):
                    mm(Pf(p), v3(Hb(mT[f // 4]), 4)[:, f % 4, ti * 128:(ti + 1) * 128], wb[:, f, 0:512], f == 0, f == 7,
                       [HP.tr(mT[f // 4]), wtr], PS.tr(p))
                tt(Ff(xs), Ff(xs), Pf(p), ALU.add, [FP.tr(xs), PS.tr(p)], [FP.tr(xs)])
                PS.free(p)
                K.dma("sp", y_d[T * 128:(T + 1) * 128, half * 512:(half + 1) * 512], Ff(xs), reads=[FP.tr(xs)], writes=[ytr[T]])
                FP.free(xs)
            w_done()
        HP.free(mT[0])
        HP.free(mT[1])

    skp = sb("skp", [128, 16], BF16); skp_t = Tr()

    def skip_blocks(keys):
        for key in keys:
            wb, wtr = w_get(key)
            K.op("dve", lambda e: e.tensor_copy(out=skp[:, :], in_=wb[:, 0, 0:16]), reads=[wtr], writes=[skp_t])
            w_done()

    init_const()
    gcount = 0
    r_done = False
    for l in range(NL):
        init_layer(l)
        cur_h["i"] = gcount % 2
        if "R" in stages and not r_done:
            init_R(l)
            r0 = stage_R(l, 0)
            if l == 0:
                convert_blocks(0, NB)
            r0()
        elif l == 0:
            convert_blocks(0, NB)
        r_done = False
        for gi in range(NG):
            cur_h["i"] = gcount % 2
            full = all(c in stages for c in "ABCD")
            ctail = None
            if full:
                gu, vln = stage_A1(l, gi)
                raws = stage_B1(l, gi)
                stage_A2(l, gi, gu, vln)
                btail = stage_B2(l, gi, raws)
                dgen = stage_D(l, gi)
                ctail = stage_C(l, gi, dgen, btail)
                for _ in dgen:
                    pass
            else:
                if "A" in stages:
                    gu, vln = stage_A1(l, gi)
                    stage_A2(l, gi, gu, vln)
                else:
                    skip_blocks(["A_u", "A_z", "A_v"])
                if "B" in stages:
                    raws = stage_B1(l, gi)
                    stage_B2(l, gi, raws)()
                else:
                    skip_blocks(["B_q", "B_k", "B_v", "B_z"])
                if "C" in stages:
                    stage_C(l, gi)()
                else:
                    skip_blocks(["C_qk", "C_lr", "C_z", "C_v"])
                if "D" in stages:
                    for _ in stage_D(l, gi):
                        pass
                else:
                    skip_blocks(["D_0", "D_1", "D_2", "D_3"])
            if dbg_d is not None and l == 0 and gi == dbg_group:
                for i in range(4):
                    K.dma("pool", dbg_d[i], yT[i][:, :, :].rearrange("p a b -> p (a b)"), reads=[yT_t[i]])
            if "R" in stages and (gi + 1 < NG or l + 1 < NL):
                cur_h["i"] = (gcount + 1) % 2
                if gi + 1 < NG:
                    r_pe = stage_R(l, gi + 1)
                else:
                    init_R(l + 1)
                    r_pe = stage_R(l + 1, 0)
                    r_done = True
                cur_h["i"] = gcount % 2
            else:
                r_pe = None
            if "M" in stages:
                mT = stage_M(l, gi, ctail)
            else:
                if ctail is not None:
                    ctail()
                skip_blocks(["%s_%d_%d" % (a, i, c2) for c2 in range(2) for i in range(4) for a in ("G", "P")])
                mT = [HP.alloc(), HP.alloc()]
            if r_pe is not None:
                r_pe()
            if "O" in stages:
                stage_O(l, gi, mT)
            else:
                skip_blocks(["O_0", "O_1"])
                HP.free(mT[0]); HP.free(mT[1])
            if l + 1 < NL:
                convert_blocks(l + 1, -(-NB // NG))
            gcount += 1
    assert wstate["cur"] == len(wseq)
    assert all(conv_pos[l_] == NB for l_ in range(NL)), conv_pos
    K.wait_all("sp", ytr + yT_t)
    if needed is None:
        return {k: sorted(v) for k, v in K.waited.items() if k in K.E}
    print("sbuf bytes remaining", nc.sbuf_bytes_remaining)
    print("kernel build: ops=%d waits=%d incs=%d" % (K.n_ops, K.n_waits, sum(len(v) for v in needed.values())))
    return nc


_NC_CACHE = {}


def _prep_shared(inputs):
    sh = {}
    for k in ("norm_g", "w_in", "a_ln_g", "a_ln_b", "b_q_norm_g", "b_k_norm_g", "c_gate_w2", "c_gate_b",
              "c_out_norm_g", "d_conv_w", "d_conv_b", "w_branch_out", "w_merge_gate", "b_merge_gate", "w_out"):
        sh[k] = np.ascontiguousarray(np.asarray(inputs[k], dtype=np.float32))
    sh["a_wsT"] = np.ascontiguousarray(np.transpose(np.asarray(inputs["a_spatial_w"], np.float32), (0, 1, 3, 2)))
    sh["a_spatial_b"] = np.ascontiguousarray(np.asarray(inputs["a_spatial_b"], np.float32).reshape(2, 512))
    sh.update(make_consts())
    return sh


def kernel(**inputs):
    x = np.asarray(inputs["x"], dtype=np.float32)
    nb = x.shape[0]
    if "nc" not in _NC_CACHE:
        _NC_CACHE["nc"] = build()
    nc = _NC_CACHE["nc"]
    sh = _prep_shared(inputs)
    in_maps = []
    for b in range(nb):
        m = dict(sh)
        m["x"] = np.ascontiguousarray(x[b])
        in_maps.append(m)
    res = run_bass_kernel_spmd(nc, in_maps, core_ids=list(range(nb)))
    out = np.stack([np.asarray(r["y"], dtype=np.float32) for r in res.results], axis=0)
    return out
```

```python
import math
import numpy as np
import ml_dtypes
import concourse.bass as bass
import concourse.mybir as mybir
from concourse.bass_utils import run_bass_kernel_spmd

F32 = mybir.dt.float32
BF16 = mybir.dt.bfloat16
AF = mybir.ActivationFunctionType
ALU = mybir.AluOpType
AX = mybir.AxisListType

S = 4096
D = 1024
NCOL = 7184
EPS = 1e-6
BIG = 32768.0
TG = 512
NT = TG // 128


class Tr:
    __slots__ = ("w", "r", "ro", "psum")

    def __init__(self, psum=False):
        self.w = None
        self.r = {}
        self.ro = False
        self.psum = psum


class _Eng:
    def __init__(self, name, h, sem):
        self.name = name
        self.h = h
        self.sem = sem
        self.cnt = 0
        self.seen = {}


class Sched:
    def __init__(self, nc, n_dma_slots=24, needed=None):
        self.nc = nc
        self.sems = {}
        self.E = {}
        self.needed = needed
        self.rank = None
        if needed is not None:
            self.rank = {k: {v: i + 1 for i, v in enumerate(vs)} for k, vs in needed.items()}
        self.waited = {}
        for name, h in (("pe", nc.tensor), ("act", nc.scalar), ("dve", nc.vector),
                        ("pool", nc.gpsimd), ("sp", nc.sync)):
            sem = nc.alloc_semaphore("sem_" + name)
            self.sems[name] = sem
            self.E[name] = _Eng(name, h, sem)
        self.slots = {}
        self.slot_i = {}
        for q in ("sp", "pool"):
            self.slots[q] = []
            self.slot_i[q] = 0
            for i in range(n_dma_slots):
                key = "dma_%s%d" % (q, i)
                self.sems[key] = nc.alloc_semaphore("sem_" + key)
                self.slots[q].append([key, 0])
        self.n_ops = 0
        self.n_waits = 0

    def _wait(self, e, deps):
        need = {}
        for d in deps:
            if d is None:
                continue
            k, v = d
            if need.get(k, 0) < v:
                need[k] = v
        for k, v in need.items():
            if e.name == "pe" and k == "pe":
                continue
            if e.seen.get(k, 0) >= v:
                continue
            self.waited.setdefault(k, set()).add(v)
            hv = v
            if self.rank is not None and k in self.rank:
                hv = self.rank[k][v]
            e.h.wait_ge(self.sems[k], hv)
            e.seen[k] = v
            self.n_waits += 1

    @staticmethod
    def _deps(reads, writes, ename=None):
        deps = []
        for t in reads:
            deps.append(t.w)
            if t.psum:
                deps.extend(kv for kv in t.r.items() if kv[0] != ename)
        for t in writes:
            deps.append(t.w)
            deps.extend(t.r.items())
        return deps

    @staticmethod
    def _commit(tok, reads, writes):
        k, v = tok
        for t in reads:
            if not t.ro:
                if t.r.get(k, 0) < v:
                    t.r[k] = v
        for t in writes:
            t.w = tok
            t.r = {}

    def op(self, ename, fn, reads=(), writes=()):
        e = self.E[ename]
        self._wait(e, self._deps(reads, writes, ename))
        ins = fn(e.h)
        e.cnt += 1
        if self.rank is None or e.cnt in self.rank.get(ename, {}):
            ins.then_inc(e.sem, 1)
        self._commit((ename, e.cnt), reads, writes)
        self.n_ops += 1

    def dma(self, qname, out, in_, reads=(), writes=(), **kw):
        e = self.E[qname]
        slot = self.slots[qname][self.slot_i[qname]]
        self.slot_i[qname] = (self.slot_i[qname] + 1) % len(self.slots[qname])
        deps = self._deps(reads, writes)
        if slot[1] > 0:
            deps.append((slot[0], slot[1] * 16))
        self._wait(e, deps)
        ins = e.h.dma_start(out=out, in_=in_, **kw)
        slot[1] += 1
        ins.then_inc(self.sems[slot[0]], 16)
        self._commit((slot[0], slot[1] * 16), reads, writes)
        self.n_ops += 1

    def wait_all(self, ename, trs):
        e = self.E[ename]
        deps = []
        for t in trs:
            deps.append(t.w)
            deps.extend(t.r.items())
        self._wait(e, deps)


class Pool:
    def __init__(self, name, aps):
        self.name = name
        self.items = [(ap, Tr()) for ap in aps]
        self.free_list = list(range(len(aps)))

    def alloc(self):
        assert self.free_list, "pool %s exhausted" % self.name
        return self.free_list.pop(0)

    def free(self, i):
        assert i not in self.free_list
        self.free_list.append(i)

    def ap(self, i):
        return self.items[i][0]

    def tr(self, i):
        return self.items[i][1]


def alibi_slopes():
    return [2.0 ** (-8.0 * (h + 1) / 4) for h in range(4)]


def make_consts():
    p = np.arange(128)
    c = {}
    c["c_ident"] = np.eye(128, dtype=np.float32)
    c["c_uincl"] = (p[:, None] <= p[None, :]).astype(np.float32)
    c["c_lstrict"] = (p[:, None] > p[None, :]).astype(np.float32)
    sl = alibi_slopes()
    al = np.zeros((128, 4 * 32), np.float32)
    for h in range(4):
        for idx in range(32):
            m = idx - 1
            al[:, h * 32 + idx] = sl[h] * (p - 128.0 * m)
    c["c_alibi"] = al
    cmb = np.zeros((128, 8), np.float32)
    for h in range(4):
        for qt in range(2):
            cmb[:, h * 2 + qt] = -sl[h] * (qt * 128.0 + p) - BIG
    c["c_cmb"] = cmb
    es = np.zeros((128, 17, 128), np.float32)
    for j in range(17):
        es[j, j, :] = 1.0
    c["c_esel"] = es.reshape(128, 17 * 128)
    return c


def build(NL=2, NG=8, dbg_group=None, stages="RABCDMO"):
    needed = _build(NL, NG, dbg_group, None, stages)
    return _build(NL, NG, dbg_group, needed, stages)


def _build(NL, NG, dbg_group, needed, stages="RABCDMO"):
    nc = bass.Bass("TRN2", target_bir_lowering=False)
    K = Sched(nc, needed=needed)

    def din(name, shape):
        return nc.dram_tensor(name, list(shape), F32, kind="ExternalInput").ap()

    x_d = din("x", [S, D])
    norm_g = din("norm_g", [2, D])
    w_in = din("w_in", [2, D, NCOL])
    a_ln_g = din("a_ln_g", [2, 512])
    a_ln_b = din("a_ln_b", [2, 512])
    a_wsT = din("a_wsT", [2, 4, 128, 128])
    a_sb = din("a_spatial_b", [2, 4 * 128])
    b_qg = din("b_q_norm_g", [2, 128])
    b_kg = din("b_k_norm_g", [2, 128])
    c_w2 = din("c_gate_w2", [2, 16, 256])
    c_b2 = din("c_gate_b", [2, 256])
    c_og = din("c_out_norm_g", [2, 128])
    d_cw = din("d_conv_w", [2, 3, 512])
    d_cb = din("d_conv_b", [2, 512])
    w_bo = din("w_branch_out", [2, 4, 512, D])
    w_mg = din("w_merge_gate", [2, 4, D, D])
    b_mg = din("b_merge_gate", [2, 4, D])
    w_o = din("w_out", [2, D, D])
    cst = {k: din(k, v.shape) for k, v in make_consts().items()}
    y_d = nc.dram_tensor("y", [S, D], F32, kind="ExternalOutput").ap()
    dbg_d = None
    if dbg_group is not None:
        dbg_d = nc.dram_tensor("dbg", [4, 128, 4 * TG], F32, kind="ExternalOutput").ap()
    ytr = [Tr() for _ in range(S // 128)]

    def sb(name, shape, dt):
        return nc.alloc_sbuf_tensor(name, list(shape), dt)

    kT = sb("kT", [128, 4, S], BF16); kT_t = Tr()
    vc = sb("vc", [128, 32, 4, 129], BF16); vc_t = Tr()
    hT2 = [sb("hT%d" % i, [128, 8, TG], BF16) for i in range(2)]; hT2_t = [Tr(), Tr()]
    cur_h = {"i": 0}
    yT = [sb("yT%d" % i, [128, 4, TG], BF16) for i in range(4)]
    yT_t = [Tr() for _ in range(4)]
    wring = [sb("wr%d" % i, [128, 8, 512], BF16) for i in range(3)]
    Hraw = [sb("H%d" % i, [128, 1024], F32) for i in range(6)]
    Fraw = [sb("F%d" % i, [128, 512], F32) for i in range(8)]
    HP = Pool("H", [t[:, :] for t in Hraw])
    FP = Pool("F", [t[:, :] for t in Fraw])
    PTP = Pool("PT", [sb("pt%d" % i, [128, 256], BF16)[:, :] for i in range(4)])
    psraw = [nc.alloc_psum_tensor("ps%d" % i, [128, 512], F32) for i in range(8)]
    PS = Pool("PS", [t[:, :] for t in psraw])
    for _i in range(8):
        PS.items[_i][1].psum = True

    def Hf(i):
        return HP.ap(i)

    def Hb(i):
        return HP.ap(i).bitcast(BF16)

    def Ff(i):
        return FP.ap(i)

    def Fb(i):
        return FP.ap(i).bitcast(BF16)

    def Pf(i):
        return PS.ap(i)

    def Pb(i):
        return PS.ap(i).bitcast(BF16)

    def v3(ap, a):
        return ap.rearrange("p (a b) -> p a b", a=a)

    ident_bf = sb("ident_bf", [128, 128], BF16); ident_t = Tr()
    uincl_bf = sb("uincl_bf", [128, 128], BF16); uinclb_t = Tr()
    uincl_f = sb("uincl_f", [128, 128], F32); uinclf_t = Tr()
    lstr_bf = sb("lstr_bf", [128, 128], BF16); lstrb_t = Tr()
    ones_f = sb("ones_f", [128, 128], F32); ones_t = Tr()
    alibi = sb("alibi", [128, 128], F32); alibi_t = Tr()
    cmb = sb("cmb", [128, 8], F32); cmb_t = Tr()
    esel = sb("esel", [128, 17, 128], BF16); esel_t = Tr()
    epsc = sb("epsc", [128, 1], F32); epsc_t = Tr()
    gbc = sb("gbc", [128, D], F32); gbc_t = Tr()
    lng = sb("lng", [128, 512], F32); lng_t = Tr()
    lnb = sb("lnb", [128, 512], F32); lnb_t = Tr()
    WsT = sb("WsT", [128, 4, 128], BF16); WsT_t = Tr()
    bsf = sb("bsf", [1, 512], F32); bsf_t = Tr()
    gq_col = sb("gq_col", [128, 1], F32); gq_t = Tr()
    gk_col = sb("gk_col", [128, 1], F32); gk_t = Tr()
    og_col = sb("og_col", [128, 1], F32); og_t = Tr()
    w2b = sb("w2b", [32, 256], BF16); w2b_t = Tr()
    cw = sb("cw", [128, 3, 4], F32); cw_t = Tr()
    cb = sb("cb", [128, 4], F32); cb_t = Tr()
    bmg = sb("bmg", [128, 4, 8], F32); bmg_t = Tr()
    kmT = sb("kmT", [128, 4, 16], BF16); kmT_t = Tr()
    Gs = sb("Gs", [128, 4, 17], F32); Gs_t = Tr()
    msk = sb("msk", [128, 4, 17], F32); msk_t = Tr()
    Aa = sb("Aa", [128, 4, 17], BF16); Aa_t = Tr()
    m8 = sb("m8", [128, 32], F32); m8_t = Tr()
    AT = [sb("AT%d" % i, [128, 4, 256], BF16) for i in range(2)]
    AT_t = [Tr(), Tr()]
    Sst = sb("Sst", [128, 2, 256], F32); Sst_t = Tr()
    Sbf = sb("Sbf", [128, 2, 256], BF16); Sbf_t = Tr()
    uh = sb("uh", [128, 4, 2], F32); uh_t = Tr()
    lrT = sb("lrT", [32, TG], BF16); lrT_t = Tr()
    st16 = sb("st16", [128, 16], F32); st16_t = Tr()
    rs16 = sb("rs16", [128, 16], F32); rs16_t = Tr()
    st16q = sb("st16q", [128, 16], F32); st16q_t = Tr()
    st16k = sb("st16k", [128, 16], F32); st16k_t = Tr()
    rs16q = sb("rs16q", [128, 16], F32); rs16q_t = Tr()
    rs16k = sb("rs16k", [128, 16], F32); rs16k_t = Tr()
    rs16a = sb("rs16a", [128, 4], F32); rs16a_t = Tr()
    st16a = sb("st16a", [128, 4], F32); st16a_t = Tr()
    Aas = [sb("Aas%d" % i, [128, 4, 17], BF16) for i in range(NT)]; Aas_t = [Tr() for _ in range(NT)]
    bnst = sb("bnst", [128, 4, 6], F32); bnst_t = Tr()
    bnmv = sb("bnmv", [128, 4, 2], F32); bnmv_t = Tr()
    kms = sb("kms", [128, 4], F32); kms_t = Tr()
    rc2 = sb("rc2", [128, 2], F32); rc2_t = Tr()
    dec2s = [sb("dec2_%d" % i, [128, 2], F32) for i in range(NT)]; dec2s_t = [Tr() for _ in range(NT)]
    qtls = [sb("qtl%d" % i, [128, 2, 128], BF16) for i in range(NT)]; qtls_t = [Tr() for _ in range(NT)]
    ktls = [sb("ktl%d" % i, [128, 2, 128], BF16) for i in range(NT)]; ktls_t = [Tr() for _ in range(NT)]
    kdecs = [sb("kdec%d" % i, [128, 256], BF16) for i in range(NT)]; kdecs_t = [Tr() for _ in range(NT)]
    attTs = [sb("attT%d" % i, [128, 4, 128], BF16) for i in range(NT)]; attTs_t = [Tr() for _ in range(NT)]

    def mm(out, lhsT, rhs, start, stop, reads, wtr):
        K.op("pe", lambda e: e.matmul(out, lhsT=lhsT, rhs=rhs, start=start, stop=stop),
             reads=reads, writes=[wtr])

    def tp(out, in_, reads, wtr):
        K.op("pe", lambda e: e.transpose(out=out, in_=in_, identity=ident_bf[:, :]),
             reads=list(reads) + [ident_t], writes=[wtr])

    def act(out, in_, func, reads, writes, **kw):
        K.op("act", lambda e: e.activation(out=out, in_=in_, func=func, **kw),
             reads=reads, writes=writes)

    def tt(out, in0, in1, op, reads, writes, eng="dve"):
        K.op(eng, lambda e: e.tensor_tensor(out=out, in0=in0, in1=in1, op=op),
             reads=reads, writes=writes)

    def ts(out, in0, s1, s2, op0, op1, reads, writes, eng="dve"):
        if op1 is None:
            K.op(eng, lambda e: e.tensor_scalar(out=out, in0=in0, scalar1=s1, scalar2=None, op0=op0),
                 reads=reads, writes=writes)
        else:
            K.op(eng, lambda e: e.tensor_scalar(out=out, in0=in0, scalar1=s1, scalar2=s2, op0=op0, op1=op1),
                 reads=reads, writes=writes)

    def stt(out, in0, scalar, in1, op0, op1, reads, writes):
        K.op("dve", lambda e: e.scalar_tensor_tensor(out=out, in0=in0, scalar=scalar, in1=in1, op0=op0, op1=op1),
             reads=reads, writes=writes)

    def rsqrt_to(dst, dst_t, src, src_t, n, scale):
        act(dst[:, 0:n], src[:, 0:n], AF.Ln, [src_t, epsc_t], [dst_t], bias=epsc[:, 0:1], scale=scale)
        act(dst[:, 0:n], dst[:, 0:n], AF.Exp, [dst_t], [dst_t], scale=-0.5)

    def rsqrt_cols(n, scale):
        act(rs16[:, 0:n], st16[:, 0:n], AF.Ln, [st16_t, epsc_t], [rs16_t], bias=epsc[:, 0:1], scale=scale)
        act(rs16[:, 0:n], rs16[:, 0:n], AF.Exp, [rs16_t], [rs16_t], scale=-0.5)

    def wview(ap2d, kk):
        return ap2d.rearrange("(k p) c -> p k c", p=128)

    def layer_blocks(l):
        win = wview(w_in[l], 8)
        out = []

        def blk(key, c0, n):
            out.append((key, [(0, win[:, :, c0:c0 + n], 8, n)], 8, n))
        blk("A_u", 0, 512); blk("A_z", 1024, 512); blk("A_v", 512, 512)
        blk("B_q", 1536, 512); blk("B_k", 2048, 512); blk("B_v", 2560, 512); blk("B_z", 3072, 512)
        blk("C_qk", 3584, 512); blk("C_lr", 4608, 16); blk("C_z", 4624, 512); blk("C_v", 4096, 512)
        for c in range(4):
            parts = []
            for qi, base in enumerate((5648, 6160, 5136, 6672)):
                parts.append((qi * 128, win[:, :, base + c * 128: base + (c + 1) * 128], 8, 128))
            out.append(("D_%d" % c, parts, 8, 512))
        for c2 in range(2):
            for i in range(4):
                out.append(("G_%d_%d" % (i, c2), [(0, wview(w_mg[l, i], 8)[:, :, c2 * 512:(c2 + 1) * 512], 8, 512)], 8, 512))
                out.append(("P_%d_%d" % (i, c2), [(0, wview(w_bo[l, i], 4)[:, :, c2 * 512:(c2 + 1) * 512], 4, 512)], 4, 512))
        for half in range(2):
            out.append(("O_%d" % half, [(0, wview(w_o[l], 8)[:, :, half * 512:(half + 1) * 512], 8, 512)], 8, 512))
        return out

    LB = [layer_blocks(l) for l in range(NL)]
    NB = len(LB[0])
    wsc = nc.dram_tensor("wsc", [NL * NB, 128, 4096], BF16).ap()
    wsc_t = [[Tr() for _ in range(NB)] for _ in range(NL)]
    conv_hist = []
    conv_pos = [0 for _ in range(NL)]

    def wsc_view(l, bi, kk, ntot):
        return wsc[l * NB + bi][:, 0:kk * ntot].rearrange("p (k c) -> p k c", k=kk)

    def convert_blocks(l, count):
        for _ in range(count):
            bi = conv_pos[l]
            if bi >= NB:
                return
            conv_pos[l] += 1
            key, parts, kk, ntot = LB[l][bi]
            dst3 = wsc_view(l, bi, kk, ntot)
            for (c0, src, kk_, n) in parts:
                thr = [conv_hist[-4]] if len(conv_hist) >= 4 else []
                tmp = Tr()
                K.dma("pool", dst3[:, :, c0:c0 + n], src, reads=thr, writes=[wsc_t[l][bi], tmp])
                conv_hist.append(tmp)

    wseq = []
    for l in range(NL):
        for gi in range(NG):
            for bi, (key, parts, kk, ntot) in enumerate(LB[l]):
                wseq.append((key, l, bi, kk, ntot))

    WP = Pool("W", [t[:, :, :] for t in wring])
    wstate = {"issued": 0, "cur": 0, "slot": {}}

    def w_ensure(upto):
        while wstate["issued"] <= upto and wstate["issued"] < len(wseq) and WP.free_list:
            i = wstate["issued"]
            s = WP.alloc()
            key, l_, bi, kk, ntot = wseq[i]
            K.dma("sp", WP.ap(s)[:, 0:kk, 0:ntot], wsc_view(l_, bi, kk, ntot), reads=[wsc_t[l_][bi]], writes=[WP.tr(s)])
            wstate["slot"][i] = s
            wstate["issued"] += 1

    def w_get(key):
        i = wstate["cur"]
        assert wseq[i][0] == key, (wseq[i][0], key)
        w_ensure(i + 2)
        s = wstate["slot"][i]
        return WP.ap(s), WP.tr(s)

    def w_done():
        i = wstate["cur"]
        WP.free(wstate["slot"].pop(i))
        wstate["cur"] += 1
        w_ensure(wstate["cur"] + 2)

    def init_const():
        K.dma("pool", ident_bf[:, :], cst["c_ident"], writes=[ident_t])
        K.dma("pool", uincl_bf[:, :], cst["c_uincl"], writes=[uinclb_t])
        K.dma("sp", uincl_f[:, :], cst["c_uincl"], writes=[uinclf_t])
        K.dma("pool", lstr_bf[:, :], cst["c_lstrict"], writes=[lstrb_t])
        K.dma("sp", alibi[:, :], cst["c_alibi"], writes=[alibi_t])
        K.dma("sp", cmb[:, :], cst["c_cmb"], writes=[cmb_t])
        for j in range(17):
            K.dma("pool", esel[:, j, :], cst["c_esel"][:, j * 128:(j + 1) * 128], writes=[esel_t])
        K.op("dve", lambda e: e.memset(ones_f[:, :], 1.0), writes=[ones_t])
        K.op("dve", lambda e: e.memset(epsc[:, :], EPS), writes=[epsc_t])
        K.op("dve", lambda e: e.memset(lrT[:, :], 1.0), writes=[lrT_t])
        for i_ in range(2):
            K.op("dve", lambda e: e.memset(AT[i_][:, :, :], 0.0), writes=[AT_t[i_]])
        K.op("dve", lambda e: e.memset(vc[:, :, :, 128:129], 1.0), writes=[vc_t])
        for t in (ident_t, uinclb_t, uinclf_t, lstrb_t, alibi_t, cmb_t, esel_t, ones_t, epsc_t):
            t.ro = True

    def init_R(l):
        K.dma("sp", gbc[:, :], norm_g[l:l + 1, :].broadcast_to([128, D]), writes=[gbc_t])

    def init_layer(l):
        K.dma("sp", lng[:, :], a_ln_g[l:l + 1, :].broadcast_to([128, 512]), writes=[lng_t])
        K.dma("sp", lnb[:, :], a_ln_b[l:l + 1, :].broadcast_to([128, 512]), writes=[lnb_t])
        K.dma("sp", bsf[:, :], a_sb[l:l + 1, :], writes=[bsf_t])
        f = FP.alloc()
        for g in range(4):
            K.dma("sp", Ff(f)[:, g * 128:(g + 1) * 128], a_wsT[l, g], writes=[FP.tr(f)])
        tt(WsT[:, :, :], v3(Ff(f), 4), uincl_f[:, :].unsqueeze(1).broadcast_to([128, 4, 128]), ALU.mult,
           [FP.tr(f), uinclf_t], [WsT_t])
        FP.free(f)
        with nc.allow_non_contiguous_dma(reason="tiny per-layer parameter columns"):
            K.dma("sp", gq_col[:, :], b_qg[l:l + 1, :].rearrange("o d -> d o"), writes=[gq_t])
            K.dma("sp", gk_col[:, :], b_kg[l:l + 1, :].rearrange("o d -> d o"), writes=[gk_t])
            K.dma("sp", og_col[:, :], c_og[l:l + 1, :].rearrange("o d -> d o"), writes=[og_t])
            for j in range(3):
                K.dma("sp", cw[:, j, :], d_cw[l, j:j + 1, :].rearrange("o (c p) -> p (o c)", p=128), writes=[cw_t])
            K.dma("sp", cb[:, :], d_cb[l:l + 1, :].rearrange("o (c p) -> p (o c)", p=128), writes=[cb_t])
            for i in range(4):
                K.dma("sp", bmg[:, i, :], b_mg[l, i:i + 1, :].rearrange("o (f p) -> p (o f)", p=128), writes=[bmg_t])
        ts(gq_col[:, :], gq_col[:, :], 128.0 ** -0.5, None, ALU.mult, None, [gq_t], [gq_t])
        K.dma("pool", w2b[0:16, :], c_w2[l], writes=[w2b_t])
        K.dma("pool", w2b[16:17, :], c_b2[l:l + 1, :], writes=[w2b_t])
        K.op("dve", lambda e: e.memset(Sst[:, :, :], 0.0), writes=[Sst_t])
        K.op("dve", lambda e: e.memset(Sbf[:, :, :], 0.0), writes=[Sbf_t])
        K.op("dve", lambda e: e.memset(uh[:, :, :], 0.0), writes=[uh_t])
        K.op("dve", lambda e: e.memset(kmT[:, :, :], 0.0), writes=[kmT_t])
        K.op("dve", lambda e: e.memset(Gs[:, :, 0:16], -1.0e30), writes=[Gs_t])
        K.op("dve", lambda e: e.memset(Gs[:, :, 16:17], 1.0e30), writes=[Gs_t])

    def proj_fm(wb, wtr, c0, evac, nparts=128, kk=8, rhs_of=None, rhs_tr=None):
        p = PS.alloc()
        for k in range(kk):
            rhs = hT2[cur_h["i"]][:, k, :] if rhs_of is None else rhs_of(k)
            mm(Pf(p)[0:nparts, 0:TG], wb[:, k, c0:c0 + nparts], rhs, k == 0, k == kk - 1,
               [wtr, hT2_t[cur_h["i"]] if rhs_tr is None else rhs_tr], PS.tr(p))
        evac(p)
        PS.free(p)

    def proj_tm(wb, wtr, ti, c0, n, evac):
        p = PS.alloc()
        for k in range(8):
            mm(Pf(p)[:, 0:n], hT2[cur_h["i"]][:, k, ti * 128:(ti + 1) * 128], wb[:, k, c0:c0 + n], k == 0, k == 7,
               [wtr, hT2_t[cur_h["i"]]], PS.tr(p))
        evac(p)
        PS.free(p)

    def stage_R(l, gi):
        src = x_d if l == 0 else y_d
        xs = []
        for ti in range(NT):
            T = gi * NT + ti
            h = HP.alloc()
            xs.append(h)
            K.dma("pool", Hf(h), src[T * 128:(T + 1) * 128, :], reads=([ytr[T]] if l > 0 else []), writes=[HP.tr(h)])
            f = FP.alloc()
            act(Fb(f)[:, 0:D], Hf(h), AF.Square, [HP.tr(h)], [FP.tr(f), st16_t], accum_out=st16[:, ti:ti + 1])
            FP.free(f)
        rsqrt_cols(NT, 1.0 / D)
        hbuf = hT2[cur_h["i"]]
        hbuf_t = hT2_t[cur_h["i"]]
        fs = []
        for ti in range(NT):
            h = xs[ti]
            f = FP.alloc()
            stt(Fb(f)[:, 0:D], Hf(h), rs16[:, ti:ti + 1], gbc[:, :], ALU.mult, ALU.mult,
                [HP.tr(h), rs16_t, gbc_t], [FP.tr(f)])
            HP.free(h)
            fs.append(f)

        def pe_part():
            for ti in range(NT):
                f = fs[ti]
                p = PS.alloc()
                for c in range(8):
                    tp(Pb(p)[:, c * 128:(c + 1) * 128], Fb(f)[:, c * 128:(c + 1) * 128], [FP.tr(f)], PS.tr(p))
                K.op("act", lambda e: e.copy(out=hbuf[:, :, ti * 128:(ti + 1) * 128], in_=v3(Pb(p), 8)),
                     reads=[PS.tr(p)], writes=[hbuf_t])
                PS.free(p)
                FP.free(f)
        return pe_part

    def stage_A1(l, gi):
        wb, wtr = w_get("A_u")
        gu = HP.alloc()
        for c in range(4):
            proj_fm(wb, wtr, c * 128, lambda p: act(v3(Hb(gu), 4)[:, c, :], Pf(p)[:, 0:TG], AF.Gelu_apprx_tanh,
                                                    [PS.tr(p)], [HP.tr(gu)]))
        w_done()
        wb, wtr = w_get("A_z")
        sz = HP.alloc()
        for c in range(4):
            proj_fm(wb, wtr, c * 128, lambda p: act(v3(Hb(sz), 4)[:, c, :], Pf(p)[:, 0:TG], AF.Silu,
                                                    [PS.tr(p)], [HP.tr(sz)]))
        w_done()
        tt(Hb(gu), Hb(gu), Hb(sz), ALU.mult, [HP.tr(gu), HP.tr(sz)], [HP.tr(gu)])
        HP.free(sz)
        wb, wtr = w_get("A_v")
        gv = []
        for ti in range(NT):
            f = FP.alloc()
            gv.append(f)
            proj_tm(wb, wtr, ti, 0, 512, lambda p: act(Ff(f), Pf(p), AF.Gelu_apprx_tanh, [PS.tr(p)], [FP.tr(f)]))
            K.op("dve", lambda e: e.bn_stats(out=bnst[:, ti, :], in_=Ff(f)), reads=[FP.tr(f)], writes=[bnst_t])
            K.op("dve", lambda e: e.bn_aggr(out=bnmv[:, ti, :], in_=bnst[:, ti, :]), reads=[bnst_t], writes=[bnmv_t])
        w_done()
        K.op("dve", lambda e: e.tensor_copy(out=st16a[:, 0:NT], in_=bnmv[:, :, 1]), reads=[bnmv_t], writes=[st16a_t])
        rsqrt_to(rs16a, rs16a_t, st16a, st16a_t, NT, 1.0)
        vln = []
        for ti in range(NT):
            f = gv[ti]
            ts(Ff(f), Ff(f), bnmv[:, ti, 0:1], rs16a[:, ti:ti + 1], ALU.subtract, ALU.mult,
               [FP.tr(f), bnmv_t, rs16a_t], [FP.tr(f)])
            tt(Ff(f), Ff(f), lng[:, :], ALU.mult, [FP.tr(f), lng_t], [FP.tr(f)])
            f2 = FP.alloc()
            tt(Fb(f2)[:, 0:512], Ff(f), lnb[:, :], ALU.add, [FP.tr(f), lnb_t], [FP.tr(f2)])
            FP.free(f)
            vln.append(f2)
        return gu, vln

    def stage_A2(l, gi, gu, vln):
        for ti in range(NT):
            f2 = vln[ti]
            p = PS.alloc()
            for g in range(4):
                mm(Pf(p)[:, g * 128:(g + 1) * 128], Fb(f2)[:, g * 128:(g + 1) * 128], WsT[:, g, :], True, False,
                   [FP.tr(f2), WsT_t], PS.tr(p))
                mm(Pf(p)[:, g * 128:(g + 1) * 128], ones_f[0:1, 0:128], bsf[0:1, g * 128:(g + 1) * 128], False, True,
                   [ones_t, bsf_t], PS.tr(p))
            tt(yT[0][:, :, ti * 128:(ti + 1) * 128], v3(Pf(p), 4), v3(Hb(gu), 4)[:, :, ti * 128:(ti + 1) * 128], ALU.mult,
               [PS.tr(p), HP.tr(gu)], [yT_t[0]])
            PS.free(p)
            FP.free(f2)
        HP.free(gu)

    def stage_B1(l, gi):
        raws = {}
        for key, ssq, ssq_t in (("B_q", st16q, st16q_t), ("B_k", st16k, st16k_t)):
            wb, wtr = w_get(key)
            hs = [HP.alloc(), HP.alloc()]
            raws[key] = hs
            for ti in range(NT):
                dst = v3(Hf(hs[ti // 2]), 2)[:, ti % 2, :]
                dtr = HP.tr(hs[ti // 2])
                proj_tm(wb, wtr, ti, 0, 512, lambda p: K.op("act", lambda e: e.copy(out=dst, in_=Pf(p)),
                                                             reads=[PS.tr(p)], writes=[dtr]))
                f2 = FP.alloc()
                tt(Ff(f2), dst, dst, ALU.mult, [dtr], [FP.tr(f2)])
                K.op("dve", lambda e: e.tensor_reduce(out=ssq[:, ti * 4:(ti + 1) * 4], in_=v3(Ff(f2), 4), axis=AX.X, op=ALU.add),
                     reads=[FP.tr(f2)], writes=[ssq_t])
                FP.free(f2)
            w_done()
        return raws

    def stage_B2(l, gi, raws):
        qT = HP.alloc()
        qTv = v3(Hb(qT), 4)
        rsqrt_to(rs16q, rs16q_t, st16q, st16q_t, 16, 1.0 / 128)
        rsqrt_to(rs16k, rs16k_t, st16k, st16k_t, 16, 1.0 / 128)
        wb, wtr = w_get("B_v")
        for ti in range(NT):
            T = gi * NT + ti
            proj_tm(wb, wtr, ti, 0, 512, lambda p: K.op("act", lambda e: e.copy(out=vc[:, T, :, 0:128], in_=v3(Pf(p), 4)),
                                                         reads=[PS.tr(p)], writes=[vc_t]))
        w_done()
        for key, ssq, ssq_t, rsx, rsx_t, gcol, gtr, dst_fn, dst_tr in (
                ("B_q", st16q, st16q_t, rs16q, rs16q_t, gq_col, gq_t,
                 lambda ti: qTv[:, :, ti * 128:(ti + 1) * 128], HP.tr(qT)),
                ("B_k", st16k, st16k_t, rs16k, rs16k_t, gk_col, gk_t,
                 lambda ti: kT[:, :, (gi * NT + ti) * 128:(gi * NT + ti + 1) * 128], kT_t)):
            hs = raws[key]
            for ti in range(NT):
                src = v3(Hf(hs[ti // 2]), 2)[:, ti % 2, :]
                f2 = FP.alloc()
                tt(v3(Fb(f2)[:, 0:512], 4), v3(src, 4), rsx[:, ti * 4:(ti + 1) * 4].unsqueeze(2).broadcast_to([128, 4, 128]),
                   ALU.mult, [HP.tr(hs[ti // 2]), rsx_t], [FP.tr(f2)])
                p = PS.alloc()
                for h in range(4):
                    tp(Pb(p)[:, h * 128:(h + 1) * 128], Fb(f2)[:, h * 128:(h + 1) * 128], [FP.tr(f2)], PS.tr(p))
                act(dst_fn(ti), v3(Pb(p)[:, 0:512], 4), AF.Identity, [PS.tr(p), gtr], [dst_tr], scale=gcol[:, 0:1])
                PS.free(p)
                FP.free(f2)
            HP.free(hs[0])
            HP.free(hs[1])
        for bl in range(2):
            B = gi * 2 + bl
            K.op("dve", lambda e: e.tensor_reduce(out=kms[:, :], in_=kT[:, :, B * 256:(B + 1) * 256], axis=AX.X, op=ALU.add),
                 reads=[kT_t], writes=[kms_t])
            ts(kmT[:, :, B], kms[:, :], 1.0 / 256, None, ALU.mult, None, [kms_t], [kmT_t])
        def topk_tile(ti):
            T = gi * NT + ti
            B = T // 2
            par = T % 2
            if B > 0:
                p = PS.alloc()
                for h in range(4):
                    mm(Pf(p)[:, h * 16:(h + 1) * 16], qTv[:, h, ti * 128:(ti + 1) * 128], kmT[:, h, 0:16], True, True,
                       [HP.tr(qT), kmT_t], PS.tr(p))
                K.op("dve", lambda e: e.tensor_copy(out=Gs[:, :, 0:B], in_=v3(Pf(p)[:, 0:64], 4)[:, :, 0:B]),
                     reads=[PS.tr(p)], writes=[Gs_t])
                PS.free(p)
            for h in range(4):
                K.op("dve", lambda e: e.max(out=m8[:, h * 8:(h + 1) * 8], in_=Gs[:, h, 0:16]), reads=[Gs_t], writes=[m8_t])
            for h in range(4):
                ts(msk[:, h, :], Gs[:, h, :], m8[:, h * 8 + 2:h * 8 + 3], None, ALU.is_ge, None, [Gs_t, m8_t], [msk_t])
            for h in range(4):
                ts(Aas[ti][:, h, :], msk[:, h, :], BIG, cmb[:, h * 2 + par:h * 2 + par + 1], ALU.mult, ALU.add,
                   [msk_t, cmb_t], [Aas_t[ti]])

        def at_tile(ti):
            T = gi * NT + ti
            B = T // 2
            par = T % 2
            at = AT[B % 2]
            at_t = AT_t[B % 2]
            p = PS.alloc()
            for h in range(4):
                tp(Pb(p)[0:17, h * 128:(h + 1) * 128], Aas[ti][:, h, :], [Aas_t[ti]], PS.tr(p))
            K.op("act", lambda e: e.copy(out=at[0:17, :, par * 128:(par + 1) * 128], in_=v3(Pb(p)[0:17, 0:512], 4)),
                 reads=[PS.tr(p)], writes=[at_t])
            PS.free(p)

        topk_tile(0)
        topk_tile(1)
        wb, wtr = w_get("B_z")
        szb = HP.alloc()
        for c in range(4):
            proj_fm(wb, wtr, c * 128, lambda p: act(v3(Hb(szb), 4)[:, c, :], Pf(p)[:, 0:TG], AF.Silu,
                                                    [PS.tr(p)], [HP.tr(szb)]))
        w_done()
        at_tile(0)
        at_tile(1)
        topk_tile(2)
        topk_tile(3)
        steps = []
        for bl in range(2):
            for h in range(4):
                for kt in range(2 * (gi * 2 + bl) + 2):
                    steps.append((bl, h, kt))
        LA = 2
        obs = [FP.alloc(), FP.alloc()]
        obvs = [Fb(o).rearrange("p (q h d) -> p q h d", q=2, h=4) for o in obs]
        issued = {}
        accs = {}

        first_b1 = 4 * (2 * (gi * 2) + 2)

        def issue(i):
            if i == first_b1:
                at_tile(2)
                at_tile(3)
            bl, h, kt = steps[i]
            B = gi * 2 + bl
            at, at_t = AT[B % 2], AT_t[B % 2]
            qloc = bl * 256
            j = kt // 2
            qs_ = 0 if kt <= 2 * B else 128
            n = 256 - qs_
            pss = PS.alloc()
            mm(Pf(pss)[:, 0:n], kT[:, h, kt * 128:(kt + 1) * 128], qTv[:, h, qloc + qs_:qloc + 256], True, False,
               [kT_t, HP.tr(qT)], PS.tr(pss))
            jsel = j if j < B else 16
            mm(Pf(pss)[:, 0:n], esel[:, jsel, :], at[:, h, qs_:256], False, True, [esel_t, at_t], PS.tr(pss))
            issued[i] = (pss, n, qs_)

        def finish_block(bl):
            for qt in range(2):
                tcol = (bl * 2 + qt) * 128
                p = PS.alloc()
                for h in range(4):
                    tp(Pb(p)[:, h * 128:(h + 1) * 128], obvs[bl][:, qt, h, :], [FP.tr(obs[bl])], PS.tr(p))
                tt(yT[1][:, :, tcol:tcol + 128], v3(Pb(p)[:, 0:512], 4), v3(Hb(szb), 4)[:, :, tcol:tcol + 128], ALU.mult,
                   [PS.tr(p), HP.tr(szb)], [yT_t[1]])
                PS.free(p)
            FP.free(obs[bl])

        deferred = []
        for i in range(min(LA, len(steps))):
            issue(i)
        for i, (bl, h, kt) in enumerate(steps):
            if i + LA < len(steps):
                issue(i + LA)
            B = gi * 2 + bl
            nkt = 2 * B + 2
            if kt == 0:
                accs[(bl, h)] = [PS.alloc(), PS.alloc()]
            ac = accs[(bl, h)]
            pss, n, qs_ = issued.pop(i)
            pt = PTP.alloc()
            aidx = h * 32 + (2 * B - kt + 1)
            act(PTP.ap(pt)[:, 0:n], Pf(pss)[:, 0:n], AF.Exp, [PS.tr(pss), alibi_t], [PTP.tr(pt)],
                bias=alibi[:, aidx:aidx + 1], scale=1.0)
            PS.free(pss)
            if kt >= 2 * B:
                tt(PTP.ap(pt)[:, 0:128], PTP.ap(pt)[:, 0:128], uincl_bf[:, :], ALU.mult,
                   [PTP.tr(pt), uinclb_t], [PTP.tr(pt)])
            for qt in range(2):
                if kt <= 2 * B + qt:
                    c0 = qt * 128 - qs_
                    mm(Pf(ac[qt])[:, 0:129], PTP.ap(pt)[:, c0:c0 + 128], vc[:, kt, h, :], kt == 0, kt == 2 * B + qt,
                       [PTP.tr(pt), vc_t], PS.tr(ac[qt]))
            PTP.free(pt)
            if kt == nkt - 1:
                for qt in range(2):
                    a_ = ac[qt]
                    K.op("dve", lambda e: e.reciprocal(out=rc2[:, qt:qt + 1], in_=Pf(a_)[:, 128:129]),
                         reads=[PS.tr(a_)], writes=[rc2_t])
                    ts(obvs[bl][:, qt, h, :], Pf(a_)[:, 0:128], rc2[:, qt:qt + 1], None, ALU.mult, None,
                       [PS.tr(a_), rc2_t], [FP.tr(obs[bl])])
                    PS.free(a_)
                del accs[(bl, h)]
                if h == 3:
                    deferred.append((i + LA + 2, bl))
            while deferred and deferred[0][0] <= i:
                finish_block(deferred.pop(0)[1])

        def tail():
            while deferred:
                finish_block(deferred.pop(0)[1])
            HP.free(qT)
            HP.free(szb)
        return tail

    def stage_C(l, gi, dgen=None, pre=None):
        wb, wtr = w_get("C_qk")
        qf = HP.alloc()
        kf = HP.alloc()
        ktm = HP.alloc()
        for c in range(4):
            dst = v3(Hf(qf), 2)[:, c, :] if c < 2 else v3(Hf(kf), 2)[:, c - 2, :]
            dtr = HP.tr(qf) if c < 2 else HP.tr(kf)
            proj_fm(wb, wtr, c * 128, lambda p: K.op("act", lambda e: e.copy(out=dst, in_=Pf(p)[:, 0:TG]),
                                                     reads=[PS.tr(p)], writes=[dtr]))
        for ti in range(NT):
            proj_tm(wb, wtr, ti, 256, 256, lambda p: K.op("act", lambda e: e.copy(out=v3(Hf(ktm), 4)[:, ti, :], in_=Pf(p)[:, 0:256]),
                                                           reads=[PS.tr(p)], writes=[HP.tr(ktm)]))
        w_done()
        if pre is not None:
            pre()
        wb, wtr = w_get("C_lr")
        proj_fm(wb, wtr, 0, lambda p: K.op("act", lambda e: e.copy(out=lrT[0:16, :], in_=Pf(p)[0:16, 0:TG]),
                                           reads=[PS.tr(p)], writes=[lrT_t]), nparts=16)
        w_done()
        hls = []

        def p1a():
            for ti in range(NT):
                tsl = slice(ti * 128, (ti + 1) * 128)
                p = PS.alloc()
                mm(Pf(p)[:, 0:256], lrT[0:17, tsl], w2b[0:17, :], True, True, [lrT_t, w2b_t], PS.tr(p))
                lg = FP.alloc()
                act(Ff(lg)[:, 0:256], Pf(p)[:, 0:256], AF.Exp, [PS.tr(p)], [FP.tr(lg)], scale=-1.0)
                PS.free(p)
                act(Ff(lg)[:, 0:256], Ff(lg)[:, 0:256], AF.Ln, [FP.tr(lg)], [FP.tr(lg)], bias=1.0, scale=1.0)
                hl = FP.alloc()
                hlv = Fb(hl)
                K.op("dve", lambda e: e.tensor_copy(out=hlv[:, 0:256], in_=Ff(lg)[:, 0:256]), reads=[FP.tr(lg)], writes=[FP.tr(hl)])
                tt(hlv[:, 256:512], Ff(lg)[:, 0:256], hlv[:, 0:256], ALU.subtract, [FP.tr(lg), FP.tr(hl)], [FP.tr(hl)])
                FP.free(lg)
                hls.append(hl)

        def p1b():
            for ti in range(NT):
                tsl = slice(ti * 128, (ti + 1) * 128)
                dec2, dec2_t = dec2s[ti], dec2s_t[ti]
                qtl, qtl_t = qtls[ti], qtls_t[ti]
                ktl, ktl_t = ktls[ti], ktls_t[ti]
                kdec, kdec_t = kdecs[ti], kdecs_t[ti]
                hl = hls[ti]
                hlv = Fb(hl)
                p = PS.alloc()
                for c in range(2):
                    mm(Pf(p)[:, c * 128:(c + 1) * 128], hlv[:, c * 128:(c + 1) * 128], uincl_bf[:, :], True, False,
                       [FP.tr(hl), uinclb_t], PS.tr(p))
                    mm(Pf(p)[:, c * 128:(c + 1) * 128], hlv[:, 256 + c * 128:256 + (c + 1) * 128], uincl_bf[:, :], False, True,
                       [FP.tr(hl), uinclb_t], PS.tr(p))
                mm(Pf(p)[:, 256:512], lstr_bf[:, :], hlv[:, 0:256], True, False, [FP.tr(hl), lstrb_t], PS.tr(p))
                mm(Pf(p)[:, 256:512], lstr_bf[:, :], hlv[:, 256:512], False, True, [FP.tr(hl), lstrb_t], PS.tr(p))
                FP.free(hl)
                e1 = FP.alloc()
                e2 = FP.alloc()
                act(Ff(e1)[:, 0:256], Pf(p)[:, 0:256], AF.Exp, [PS.tr(p)], [FP.tr(e1)], scale=-1.0 / 16)
                act(Ff(e1)[:, 256:512], Pf(p)[:, 0:256], AF.Exp, [PS.tr(p)], [FP.tr(e1)], scale=1.0 / 16)
                act(Ff(e2)[:, 0:256], Pf(p)[:, 256:512], AF.Exp, [PS.tr(p)], [FP.tr(e2)], scale=-1.0 / 16)
                act(dec2[:, :], v3(Pf(p)[:, 0:256], 2)[:, :, 127], AF.Exp, [PS.tr(p)], [dec2_t], scale=-1.0 / 16)
                PS.free(p)
                stt(qtl[:, :, :], v3(Hf(qf), 2)[:, :, tsl], 0.125, v3(Ff(e1)[:, 0:256], 2), ALU.mult, ALU.mult,
                    [HP.tr(qf), FP.tr(e1)], [qtl_t])
                tt(ktl[:, :, :], v3(Hf(kf), 2)[:, :, tsl], v3(Ff(e1)[:, 256:512], 2), ALU.mult,
                   [HP.tr(kf), FP.tr(e1)], [ktl_t])
                tt(kdec[:, :], v3(Hf(ktm), 4)[:, ti, :], Ff(e2)[:, 0:256], ALU.mult, [HP.tr(ktm), FP.tr(e2)], [kdec_t])
                FP.free(e1)
                FP.free(e2)

        def p1c():
            for ti in range(NT):
                qtl, qtl_t = qtls[ti], qtls_t[ti]
                ktl, ktl_t = ktls[ti], ktls_t[ti]
                attT, attT_t = attTs[ti], attTs_t[ti]
                pa = [PS.alloc(), PS.alloc()]
                for h in range(4):
                    c, po = h // 2, 64 * (h % 2)
                    mm(Pf(pa[h % 2])[:, c * 128:(c + 1) * 128], ktl[po:po + 64, c, :], qtl[po:po + 64, c, :], True, True,
                       [ktl_t, qtl_t], PS.tr(pa[h % 2]))
                for r in range(2):
                    tt(attT[:, r::2, :], v3(Pf(pa[r])[:, 0:256], 2), uincl_f[:, :].unsqueeze(1).broadcast_to([128, 2, 128]), ALU.mult,
                       [PS.tr(pa[r]), uinclf_t], [attT_t])
                    PS.free(pa[r])


        p1a()
        wb, wtr = w_get("C_z")
        szc = HP.alloc()
        for c in range(4):
            proj_fm(wb, wtr, c * 128, lambda p: act(v3(Hb(szc), 4)[:, c, :], Pf(p)[:, 0:TG], AF.Silu,
                                                    [PS.tr(p)], [HP.tr(szc)]))
        w_done()
        p1b()
        wb, wtr = w_get("C_v")
        vg = HP.alloc()
        for ti in range(NT):
            proj_tm(wb, wtr, ti, 0, 512, lambda p: K.op("act", lambda e: e.copy(out=v3(Hb(vg), 4)[:, ti, :], in_=Pf(p)),
                                                         reads=[PS.tr(p)], writes=[HP.tr(vg)]))
        w_done()
        vgv = v3(Hb(vg), 4)
        p1c()

        def finish_tile(ti, sq):
            tsl = slice(ti * 128, (ti + 1) * 128)
            p = PS.alloc()
            for h in range(4):
                tp(Pb(p)[:, h * 128:(h + 1) * 128], Fb(sq)[:, h * 128:(h + 1) * 128], [FP.tr(sq)], PS.tr(p))
            stt(yT[2][:, :, tsl], v3(Pb(p)[:, 0:512], 4), og_col[:, 0:1], v3(Hb(szc), 4)[:, :, tsl], ALU.mult, ALU.mult,
                [PS.tr(p), og_t, HP.tr(szc)], [yT_t[2]])
            PS.free(p)
            FP.free(sq)

        pend = None
        for ti in range(NT):
            dec2, dec2_t = dec2s[ti], dec2s_t[ti]
            qtl, qtl_t = qtls[ti], qtls_t[ti]
            kdec, kdec_t = kdecs[ti], kdecs_t[ti]
            attT, attT_t = attTs[ti], attTs_t[ti]
            po_ = PS.alloc()
            for h in range(4):
                c, po = h // 2, 64 * (h % 2)
                mm(Pf(po_)[:, h * 128:(h + 1) * 128], attT[:, h, :], vgv[:, ti, h * 128:(h + 1) * 128], True, False,
                   [attT_t, HP.tr(vg)], PS.tr(po_))
                mm(Pf(po_)[:, h * 128:(h + 1) * 128], qtl[po:po + 64, c, :], Sbf[po:po + 64, c, (h % 2) * 128:(h % 2 + 1) * 128],
                   False, True, [qtl_t, Sbf_t], PS.tr(po_))
            p = PS.alloc()
            for c in range(2):
                mm(Pf(p)[:, c * 256:(c + 1) * 256], kdec[:, c * 128:(c + 1) * 128], vgv[:, ti, c * 256:(c + 1) * 256], True, True,
                   [kdec_t, HP.tr(vg)], PS.tr(p))
            for c in range(2):
                stt(Sst[:, c, :], Sst[:, c, :], dec2[:, c:c + 1], Pf(p)[:, c * 256:(c + 1) * 256], ALU.mult, ALU.add,
                    [Sst_t, dec2_t, PS.tr(p)], [Sst_t])
            PS.free(p)
            K.op("act", lambda e: e.copy(out=Sbf[:, :, :], in_=Sst[:, :, :]), reads=[Sst_t], writes=[Sbf_t])
            if dgen is not None:
                next(dgen, None)
            if pend is not None:
                finish_tile(*pend)
            osb = FP.alloc()
            K.op("act", lambda e: e.copy(out=Ff(osb), in_=Pf(po_)), reads=[PS.tr(po_)], writes=[FP.tr(osb)])
            PS.free(po_)
            sq = FP.alloc()
            tt(Ff(sq), Ff(osb), Ff(osb), ALU.mult, [FP.tr(osb)], [FP.tr(sq)])
            K.op("dve", lambda e: e.tensor_reduce(out=st16[:, 0:4], in_=v3(Ff(sq), 4), axis=AX.X, op=ALU.add),
                 reads=[FP.tr(sq)], writes=[st16_t])
            rsqrt_cols(4, 1.0 / 128)
            tt(v3(Fb(sq)[:, 0:512], 4), v3(Ff(osb), 4), rs16[:, 0:4].unsqueeze(2).broadcast_to([128, 4, 128]), ALU.mult,
               [FP.tr(osb), rs16_t], [FP.tr(sq)])
            FP.free(osb)
            pend = (ti, sq)
        for h_ in (qf, kf, ktm, vg):
            HP.free(h_)

        def ctail():
            finish_tile(*pend)
            HP.free(szc)
        return ctail

    def stage_D(l, gi):
        for c in range(4):
            wb, wtr = w_get("D_%d" % c)
            ps4 = []
            for qi in range(4):
                p = PS.alloc()
                for k in range(8):
                    mm(Pf(p)[:, 0:TG], wb[:, k, qi * 128:(qi + 1) * 128], hT2[cur_h["i"]][:, k, :], k == 0, k == 7, [wtr, hT2_t[cur_h["i"]]], PS.tr(p))
                ps4.append(p)
            w_done()
            p_cg, p_xin, p_bg, p_z = ps4
            u = FP.alloc()
            K.op("act", lambda e: e.copy(out=Ff(u), in_=Pf(p_cg)), reads=[PS.tr(p_cg)], writes=[FP.tr(u)])
            PS.free(p_cg)
            tt(Ff(u), Ff(u), Pf(p_xin), ALU.mult, [FP.tr(u), PS.tr(p_xin)], [FP.tr(u)])
            PS.free(p_xin)
            y = FP.alloc()
            ts(Ff(y), Ff(u), cw[:, 2, c:c + 1], cb[:, c:c + 1], ALU.mult, ALU.add, [FP.tr(u), cw_t, cb_t], [FP.tr(y)])
            stt(Ff(y)[:, 1:512], Ff(u)[:, 0:511], cw[:, 1, c:c + 1], Ff(y)[:, 1:512], ALU.mult, ALU.add,
                [FP.tr(u), cw_t, FP.tr(y)], [FP.tr(y)])
            stt(Ff(y)[:, 2:512], Ff(u)[:, 0:510], cw[:, 0, c:c + 1], Ff(y)[:, 2:512], ALU.mult, ALU.add,
                [FP.tr(u), cw_t, FP.tr(y)], [FP.tr(y)])
            stt(Ff(y)[:, 0:1], uh[:, c, 1:2], cw[:, 1, c:c + 1], Ff(y)[:, 0:1], ALU.mult, ALU.add,
                [uh_t, cw_t, FP.tr(y)], [FP.tr(y)])
            stt(Ff(y)[:, 0:2], uh[:, c, 0:2], cw[:, 0, c:c + 1], Ff(y)[:, 0:2], ALU.mult, ALU.add,
                [uh_t, cw_t, FP.tr(y)], [FP.tr(y)])
            K.op("dve", lambda e: e.tensor_copy(out=uh[:, c, :], in_=Ff(u)[:, 510:512]), reads=[FP.tr(u)], writes=[uh_t])
            FP.free(u)
            sz = FP.alloc()
            act(Ff(sz), Pf(p_z), AF.Silu, [PS.tr(p_z)], [FP.tr(sz)])
            PS.free(p_z)
            tt(Ff(y), Ff(y), Ff(sz), ALU.mult, [FP.tr(y), FP.tr(sz)], [FP.tr(y)])
            FP.free(sz)
            tt(yT[3][:, c, :], Ff(y), Pf(p_bg), ALU.mult, [FP.tr(y), PS.tr(p_bg)], [yT_t[3]])
            PS.free(p_bg)
            FP.free(y)
            yield c

    def stage_M(l, gi, mid=None):
        mT = [HP.alloc(), HP.alloc()]
        for c2 in range(2):
            ma = [HP.alloc(), HP.alloc()]

            def macc(f):
                return v3(Hf(ma[f // 2]), 2)[:, f % 2, :], HP.tr(ma[f // 2])
            for i in range(4):
                wb, wtr = w_get("G_%d_%d" % (i, c2))
                gf = [FP.alloc(), FP.alloc()]
                gh = HP.alloc()
                gaps = [Ff(gf[0]), Ff(gf[1]), v3(Hf(gh), 2)[:, 0, :], v3(Hf(gh), 2)[:, 1, :]]
                gtrs = [FP.tr(gf[0]), FP.tr(gf[1]), HP.tr(gh), HP.tr(gh)]
                for f in range(4):
                    fidx = c2 * 4 + f
                    proj_fm(wb, wtr, f * 128, lambda p: act(gaps[f], Pf(p)[:, 0:TG], AF.Sigmoid, [PS.tr(p), bmg_t], [gtrs[f]],
                                                            bias=bmg[:, i, fidx:fidx + 1], scale=1.0))
                w_done()
                wb, wtr = w_get("P_%d_%d" % (i, c2))
                for f in range(4):
                    gap, gtr_ = gaps[f], gtrs[f]
                    mac, mtr = macc(f)

                    def ev(p):
                        if i == 0:
                            tt(mac, gap, Pf(p)[:, 0:TG], ALU.mult, [gtr_, PS.tr(p)], [mtr])
                        else:
                            tt(gap, gap, Pf(p)[:, 0:TG], ALU.mult, [gtr_, PS.tr(p)], [gtr_])
                            if i < 3:
                                tt(mac, mac, gap, ALU.add, [mtr, gtr_], [mtr])
                            else:
                                tt(v3(Hb(mT[c2]), 4)[:, f, :], mac, gap, ALU.add, [mtr, gtr_], [HP.tr(mT[c2])])
                    proj_fm(wb, wtr, f * 128, ev, kk=4, rhs_of=lambda k: yT[i][:, k, :], rhs_tr=yT_t[i])
                FP.free(gf[0])
                FP.free(gf[1])
                HP.free(gh)
                w_done()
                if mid is not None and c2 == 0 and i == 1:
                    mid()
            HP.free(ma[0])
            HP.free(ma[1])
        return mT

    def stage_O(l, gi, mT):
        for half in range(2):
            wb, wtr = w_get("O_%d" % half)
            src = x_d if l == 0 else y_d
            for ti in range(NT):
                T = gi * NT + ti
                xs = FP.alloc()
                K.dma("pool", Ff(xs), src[T * 128:(T + 1) * 128, half * 512:(half + 1) * 512],
                      reads=([ytr[T]] if l > 0 else []), writes=[FP.tr(xs)])
                p = PS.alloc()
                for f in range(8):
                    mm(Pf(p), v3(Hb(mT[f // 4]), 4)[:, f % 4, ti * 128:(ti + 1) * 128], wb[:, f, 0:512], f == 0, f == 7,
                       [HP.tr(mT[f // 4]), wtr], PS.tr(p))
                tt(Ff(xs), Ff(xs), Pf(p), ALU.add, [FP.tr(xs), PS.tr(p)], [FP.tr(xs)])
                PS.free(p)
                K.dma("sp", y_d[T * 128:(T + 1) * 128, half * 512:(half + 1) * 512], Ff(xs), reads=[FP.tr(xs)], writes=[ytr[T]])
                FP.free(xs)
            w_done()
        HP.free(mT[0])
        HP.free(mT[1])

    skp = sb("skp", [128, 16], BF16); skp_t = Tr()

    def skip_blocks(keys):
        for key in keys:
            wb, wtr = w_get(key)
            K.op("dve", lambda e: e.tensor_copy(out=skp[:, :], in_=wb[:, 0, 0:16]), reads=[wtr], writes=[skp_t])
            w_done()

    init_const()
    gcount = 0
    r_done = False
    for l in range(NL):
        init_layer(l)
        cur_h["i"] = gcount % 2
        if "R" in stages and not r_done:
            init_R(l)
            r0 = stage_R(l, 0)
            if l == 0:
                convert_blocks(0, NB)
            r0()
        elif l == 0:
            convert_blocks(0, NB)
        r_done = False
        for gi in range(NG):
            cur_h["i"] = gcount % 2
            full = all(c in stages for c in "ABCD")
            ctail = None
            if full:
                gu, vln = stage_A1(l, gi)
                raws = stage_B1(l, gi)
                stage_A2(l, gi, gu, vln)
                btail = stage_B2(l, gi, raws)
                dgen = stage_D(l, gi)
                ctail = stage_C(l, gi, dgen, btail)
                for _ in dgen:
                    pass
            else:
                if "A" in stages:
                    gu, vln = stage_A1(l, gi)
                    stage_A2(l, gi, gu, vln)
                else:
                    skip_blocks(["A_u", "A_z", "A_v"])
                if "B" in stages:
                    raws = stage_B1(l, gi)
                    stage_B2(l, gi, raws)()
                else:
                    skip_blocks(["B_q", "B_k", "B_v", "B_z"])
                if "C" in stages:
                    stage_C(l, gi)()
                else:
                    skip_blocks(["C_qk", "C_lr", "C_z", "C_v"])
                if "D" in stages:
                    for _ in stage_D(l, gi):
                        pass
                else:
                    skip_blocks(["D_0", "D_1", "D_2", "D_3"])
            if dbg_d is not None and l == 0 and gi == dbg_group:
                for i in range(4):
                    K.dma("pool", dbg_d[i], yT[i][:, :, :].rearrange("p a b -> p (a b)"), reads=[yT_t[i]])
            if "R" in stages and (gi + 1 < NG or l + 1 < NL):
                cur_h["i"] = (gcount + 1) % 2
                if gi + 1 < NG:
                    r_pe = stage_R(l, gi + 1)
                else:
                    init_R(l + 1)
                    r_pe = stage_R(l + 1, 0)
                    r_done = True
                cur_h["i"] = gcount % 2
            else:
                r_pe = None
            if "M" in stages:
                mT = stage_M(l, gi, ctail)
            else:
                if ctail is not None:
                    ctail()
                skip_blocks(["%s_%d_%d" % (a, i, c2) for c2 in range(2) for i in range(4) for a in ("G", "P")])
                mT = [HP.alloc(), HP.alloc()]
            if r_pe is not None:
                r_pe()
            if "O" in stages:
                stage_O(l, gi, mT)
            else:
                skip_blocks(["O_0", "O_1"])
                HP.free(mT[0]); HP.free(mT[1])
            if l + 1 < NL:
                convert_blocks(l + 1, -(-NB // NG))
            gcount += 1
    assert wstate["cur"] == len(wseq)
    assert all(conv_pos[l_] == NB for l_ in range(NL)), conv_pos
    K.wait_all("sp", ytr + yT_t)
    if needed is None:
        return {k: sorted(v) for k, v in K.waited.items() if k in K.E}
    print("sbuf bytes remaining", nc.sbuf_bytes_remaining)
    print("kernel build: ops=%d waits=%d incs=%d" % (K.n_ops, K.n_waits, sum(len(v) for v in needed.values())))
    return nc


_NC_CACHE = {}


def _prep_shared(inputs):
    sh = {}
    for k in ("norm_g", "w_in", "a_ln_g", "a_ln_b", "b_q_norm_g", "b_k_norm_g", "c_gate_w2", "c_gate_b",
              "c_out_norm_g", "d_conv_w", "d_conv_b", "w_branch_out", "w_merge_gate", "b_merge_gate", "w_out"):
        sh[k] = np.ascontiguousarray(np.asarray(inputs[k], dtype=np.float32))
    sh["a_wsT"] = np.ascontiguousarray(np.transpose(np.asarray(inputs["a_spatial_w"], np.float32), (0, 1, 3, 2)))
    sh["a_spatial_b"] = np.ascontiguousarray(np.asarray(inputs["a_spatial_b"], np.float32).reshape(2, 512))
    sh.update(make_consts())
    return sh


def kernel(**inputs):
    x = np.asarray(inputs["x"], dtype=np.float32)
    nb = x.shape[0]
    if "nc" not in _NC_CACHE:
        _NC_CACHE["nc"] = build()
    nc = _NC_CACHE["nc"]
    sh = _prep_shared(inputs)
    in_maps = []
    for b in range(nb):
        m = dict(sh)
        m["x"] = np.ascontiguousarray(x[b])
        in_maps.append(m)
    res = run_bass_kernel_spmd(nc, in_maps, core_ids=list(range(nb)))
    out = np.stack([np.asarray(r["y"], dtype=np.float32) for r in res.results], axis=0)
    return out
```

```python
import math
import numpy as np
import ml_dtypes
import concourse.bass as bass
import concourse.mybir as mybir
from concourse.bass_utils import run_bass_kernel_spmd

F32 = mybir.dt.float32
BF16 = mybir.dt.bfloat16
AF = mybir.ActivationFunctionType
ALU = mybir.AluOpType
AX = mybir.AxisListType

S = 4096
D = 1024
NCOL = 7184
EPS = 1e-6
BIG = 32768.0
TG = 512
NT = TG // 128


class Tr:
    __slots__ = ("w", "r", "ro", "psum")

    def __init__(self, psum=False):
        self.w = None
        self.r = {}
        self.ro = False
        self.psum = psum


class _Eng:
    def __init__(self, name, h, sem):
        self.name = name
        self.h = h
        self.sem = sem
        self.cnt = 0
        self.seen = {}


class Sched:
    def __init__(self, nc, n_dma_slots=24, needed=None):
        self.nc = nc
        self.sems = {}
        self.E = {}
        self.needed = needed
        self.rank = None
        if needed is not None:
            self.rank = {k: {v: i + 1 for i, v in enumerate(vs)} for k, vs in needed.items()}
        self.waited = {}
        for name, h in (("pe", nc.tensor), ("act", nc.scalar), ("dve", nc.vector),
                        ("pool", nc.gpsimd), ("sp", nc.sync)):
            sem = nc.alloc_semaphore("sem_" + name)
            self.sems[name] = sem
            self.E[name] = _Eng(name, h, sem)
        self.slots = {}
        self.slot_i = {}
        for q in ("sp", "pool"):
            self.slots[q] = []
            self.slot_i[q] = 0
            for i in range(n_dma_slots):
                key = "dma_%s%d" % (q, i)
                self.sems[key] = nc.alloc_semaphore("sem_" + key)
                self.slots[q].append([key, 0])
        self.n_ops = 0
        self.n_waits = 0

    def _wait(self, e, deps):
        need = {}
        for d in deps:
            if d is None:
                continue
            k, v = d
            if need.get(k, 0) < v:
                need[k] = v
        for k, v in need.items():
            if e.name == "pe" and k == "pe":
                continue
            if e.seen.get(k, 0) >= v:
                continue
            self.waited.setdefault(k, set()).add(v)
            hv = v
            if self.rank is not None and k in self.rank:
                hv = self.rank[k][v]
            e.h.wait_ge(self.sems[k], hv)
            e.seen[k] = v
            self.n_waits += 1

    @staticmethod
    def _deps(reads, writes, ename=None):
        deps = []
        for t in reads:
            deps.append(t.w)
            if t.psum:
                deps.extend(kv for kv in t.r.items() if kv[0] != ename)
        for t in writes:
            deps.append(t.w)
            deps.extend(t.r.items())
        return deps

    @staticmethod
    def _commit(tok, reads, writes):
        k, v = tok
        for t in reads:
            if not t.ro:
                if t.r.get(k, 0) < v:
                    t.r[k] = v
        for t in writes:
            t.w = tok
            t.r = {}

    def op(self, ename, fn, reads=(), writes=()):
        e = self.E[ename]
        self._wait(e, self._deps(reads, writes, ename))
        ins = fn(e.h)
        e.cnt += 1
        if self.rank is None or e.cnt in self.rank.get(ename, {}):
            ins.then_inc(e.sem, 1)
        self._commit((ename, e.cnt), reads, writes)
        self.n_ops += 1

    def dma(self, qname, out, in_, reads=(), writes=(), **kw):
        e = self.E[qname]
        slot = self.slots[qname][self.slot_i[qname]]
        self.slot_i[qname] = (self.slot_i[qname] + 1) % len(self.slots[qname])
        deps = self._deps(reads, writes)
        if slot[1] > 0:
            deps.append((slot[0], slot[1] * 16))
        self._wait(e, deps)
        ins = e.h.dma_start(out=out, in_=in_, **kw)
        slot[1] += 1
        ins.then_inc(self.sems[slot[0]], 16)
        self._commit((slot[0], slot[1] * 16), reads, writes)
        self.n_ops += 1

    def wait_all(self, ename, trs):
        e = self.E[ename]
        deps = []
        for t in trs:
            deps.append(t.w)
            deps.extend(t.r.items())
        self._wait(e, deps)


class Pool:
    def __init__(self, name, aps):
        self.name = name
        self.items = [(ap, Tr()) for ap in aps]
        self.free_list = list(range(len(aps)))

    def alloc(self):
        assert self.free_list, "pool %s exhausted" % self.name
        return self.free_list.pop(0)

    def free(self, i):
        assert i not in self.free_list
        self.free_list.append(i)

    def ap(self, i):
        return self.items[i][0]

    def tr(self, i):
        return self.items[i][1]


def alibi_slopes():
    return [2.0 ** (-8.0 * (h + 1) / 4) for h in range(4)]


def make_consts():
    p = np.arange(128)
    c = {}
    c["c_ident"] = np.eye(128, dtype=np.float32)
    c["c_uincl"] = (p[:, None] <= p[None, :]).astype(np.float32)
    c["c_lstrict"] = (p[:, None] > p[None, :]).astype(np.float32)
    sl = alibi_slopes()
    al = np.zeros((128, 4 * 32), np.float32)
    for h in range(4):
        for idx in range(32):
            m = idx - 1
            al[:, h * 32 + idx] = sl[h] * (p - 128.0 * m)
    c["c_alibi"] = al
    cmb = np.zeros((128, 8), np.float32)
    for h in range(4):
        for qt in range(2):
            cmb[:, h * 2 + qt] = -sl[h] * (qt * 128.0 + p) - BIG
    c["c_cmb"] = cmb
    es = np.zeros((128, 17, 128), np.float32)
    for j in range(17):
        es[j, j, :] = 1.0
    c["c_esel"] = es.reshape(128, 17 * 128)
    return c


def build(NL=2, NG=8, dbg_group=None, stages="RABCDMO"):
    needed = _build(NL, NG, dbg_group, None, stages)
    return _build(NL, NG, dbg_group, needed, stages)


def _build(NL, NG, dbg_group, needed, stages="RABCDMO"):
    nc = bass.Bass("TRN2", target_bir_lowering=False)
    K = Sched(nc, needed=needed)

    def din(name, shape):
        return nc.dram_tensor(name, list(shape), F32, kind="ExternalInput").ap()

    x_d = din("x", [S, D])
    norm_g = din("norm_g", [2, D])
    w_in = din("w_in", [2, D, NCOL])
    a_ln_g = din("a_ln_g", [2, 512])
    a_ln_b = din("a_ln_b", [2, 512])
    a_wsT = din("a_wsT", [2, 4, 128, 128])
    a_sb = din("a_spatial_b", [2, 4 * 128])
    b_qg = din("b_q_norm_g", [2, 128])
    b_kg = din("b_k_norm_g", [2, 128])
    c_w2 = din("c_gate_w2", [2, 16, 256])
    c_b2 = din("c_gate_b", [2, 256])
    c_og = din("c_out_norm_g", [2, 128])
    d_cw = din("d_conv_w", [2, 3, 512])
    d_cb = din("d_conv_b", [2, 512])
    w_bo = din("w_branch_out", [2, 4, 512, D])
    w_mg = din("w_merge_gate", [2, 4, D, D])
    b_mg = din("b_merge_gate", [2, 4, D])
    w_o = din("w_out", [2, D, D])
    cst = {k: din(k, v.shape) for k, v in make_consts().items()}
    y_d = nc.dram_tensor("y", [S, D], F32, kind="ExternalOutput").ap()
    dbg_d = None
    if dbg_group is not None:
        dbg_d = nc.dram_tensor("dbg", [4, 128, 4 * TG], F32, kind="ExternalOutput").ap()
    ytr = [Tr() for _ in range(S // 128)]

    def sb(name, shape, dt):
        return nc.alloc_sbuf_tensor(name, list(shape), dt)

    kT = sb("kT", [128, 4, S], BF16); kT_t = Tr()
    vc = sb("vc", [128, 32, 4, 129], BF16); vc_t = Tr()
    hT2 = [sb("hT%d" % i, [128, 8, TG], BF16) for i in range(2)]; hT2_t = [Tr(), Tr()]
    cur_h = {"i": 0}
    yT = [sb("yT%d" % i, [128, 4, TG], BF16) for i in range(4)]
    yT_t = [Tr() for _ in range(4)]
    wring = [sb("wr%d" % i, [128, 8, 512], BF16) for i in range(3)]
    Hraw = [sb("H%d" % i, [128, 1024], F32) for i in range(6)]
    Fraw = [sb("F%d" % i, [128, 512], F32) for i in range(8)]
    HP = Pool("H", [t[:, :] for t in Hraw])
    FP = Pool("F", [t[:, :] for t in Fraw])
    PTP = Pool("PT", [sb("pt%d" % i, [128, 256], BF16)[:, :] for i in range(4)])
    psraw = [nc.alloc_psum_tensor("ps%d" % i, [128, 512], F32) for i in range(8)]
    PS = Pool("PS", [t[:, :] for t in psraw])
    for _i in range(8):
        PS.items[_i][1].psum = True

    def Hf(i):
        return HP.ap(i)

    def Hb(i):
        return HP.ap(i).bitcast(BF16)

    def Ff(i):
        return FP.ap(i)

    def Fb(i):
        return FP.ap(i).bitcast(BF16)

    def Pf(i):
        return PS.ap(i)

    def Pb(i):
        return PS.ap(i).bitcast(BF16)

    def v3(ap, a):
        return ap.rearrange("p (a b) -> p a b", a=a)

    ident_bf = sb("ident_bf", [128, 128], BF16); ident_t = Tr()
    uincl_bf = sb("uincl_bf", [128, 128], BF16); uinclb_t = Tr()
    uincl_f = sb("uincl_f", [128, 128], F32); uinclf_t = Tr()
    lstr_bf = sb("lstr_bf", [128, 128], BF16); lstrb_t = Tr()
    ones_f = sb("ones_f", [128, 128], F32); ones_t = Tr()
    alibi = sb("alibi", [128, 128], F32); alibi_t = Tr()
    cmb = sb("cmb", [128, 8], F32); cmb_t = Tr()
    esel = sb("esel", [128, 17, 128], BF16); esel_t = Tr()
    epsc = sb("epsc", [128, 1], F32); epsc_t = Tr()
    gbc = sb("gbc", [128, D], F32); gbc_t = Tr()
    lng = sb("lng", [128, 512], F32); lng_t = Tr()
    lnb = sb("lnb", [128, 512], F32); lnb_t = Tr()
    WsT = sb("WsT", [128, 4, 128], BF16); WsT_t = Tr()
    bsf = sb("bsf", [1, 512], F32); bsf_t = Tr()
    gq_col = sb("gq_col", [128, 1], F32); gq_t = Tr()
    gk_col = sb("gk_col", [128, 1], F32); gk_t = Tr()
    og_col = sb("og_col", [128, 1], F32); og_t = Tr()
    w2b = sb("w2b", [32, 256], BF16); w2b_t = Tr()
    cw = sb("cw", [128, 3, 4], F32); cw_t = Tr()
    cb = sb("cb", [128, 4], F32); cb_t = Tr()
    bmg = sb("bmg", [128, 4, 8], F32); bmg_t = Tr()
    kmT = sb("kmT", [128, 4, 16], BF16); kmT_t = Tr()
    Gs = sb("Gs", [128, 4, 17], F32); Gs_t = Tr()
    msk = sb("msk", [128, 4, 17], F32); msk_t = Tr()
    Aa = sb("Aa", [128, 4, 17], BF16); Aa_t = Tr()
    m8 = sb("m8", [128, 32], F32); m8_t = Tr()
    AT = [sb("AT%d" % i, [128, 4, 256], BF16) for i in range(2)]
    AT_t = [Tr(), Tr()]
    Sst = sb("Sst", [128, 2, 256], F32); Sst_t = Tr()
    Sbf = sb("Sbf", [128, 2, 256], BF16); Sbf_t = Tr()
    uh = sb("uh", [128, 4, 2], F32); uh_t = Tr()
    lrT = sb("lrT", [32, TG], BF16); lrT_t = Tr()
    st16 = sb("st16", [128, 16], F32); st16_t = Tr()
    rs16 = sb("rs16", [128, 16], F32); rs16_t = Tr()
    st16q = sb("st16q", [128, 16], F32); st16q_t = Tr()
    st16k = sb("st16k", [128, 16], F32); st16k_t = Tr()
    rs16q = sb("rs16q", [128, 16], F32); rs16q_t = Tr()
    rs16k = sb("rs16k", [128, 16], F32); rs16k_t = Tr()
    rs16a = sb("rs16a", [128, 4], F32); rs16a_t = Tr()
    st16a = sb("st16a", [128, 4], F32); st16a_t = Tr()
    Aas = [sb("Aas%d" % i, [128, 4, 17], BF16) for i in range(NT)]; Aas_t = [Tr() for _ in range(NT)]
    bnst = sb("bnst", [128, 4, 6], F32); bnst_t = Tr()
    bnmv = sb("bnmv", [128, 4, 2], F32); bnmv_t = Tr()
    kms = sb("kms", [128, 4], F32); kms_t = Tr()
    rc2 = sb("rc2", [128, 2], F32); rc2_t = Tr()
    dec2s = [sb("dec2_%d" % i, [128, 2], F32) for i in range(NT)]; dec2s_t = [Tr() for _ in range(NT)]
    qtls = [sb("qtl%d" % i, [128, 2, 128], BF16) for i in range(NT)]; qtls_t = [Tr() for _ in range(NT)]
    ktls = [sb("ktl%d" % i, [128, 2, 128], BF16) for i in range(NT)]; ktls_t = [Tr() for _ in range(NT)]
    kdecs = [sb("kdec%d" % i, [128, 256], BF16) for i in range(NT)]; kdecs_t = [Tr() for _ in range(NT)]
    attTs = [sb("attT%d" % i, [128, 4, 128], BF16) for i in range(NT)]; attTs_t = [Tr() for _ in range(NT)]

    def mm(out, lhsT, rhs, start, stop, reads, wtr):
        K.op("pe", lambda e: e.matmul(out, lhsT=lhsT, rhs=rhs, start=start, stop=stop),
             reads=reads, writes=[wtr])

    def tp(out, in_, reads, wtr):
        K.op("pe", lambda e: e.transpose(out=out, in_=in_, identity=ident_bf[:, :]),
             reads=list(reads) + [ident_t], writes=[wtr])

    def act(out, in_, func, reads, writes, **kw):
        K.op("act", lambda e: e.activation(out=out, in_=in_, func=func, **kw),
             reads=reads, writes=writes)

    def tt(out, in0, in1, op, reads, writes, eng="dve"):
        K.op(eng, lambda e: e.tensor_tensor(out=out, in0=in0, in1=in1, op=op),
             reads=reads, writes=writes)

    def ts(out, in0, s1, s2, op0, op1, reads, writes, eng="dve"):
        if op1 is None:
            K.op(eng, lambda e: e.tensor_scalar(out=out, in0=in0, scalar1=s1, scalar2=None, op0=op0),
                 reads=reads, writes=writes)
        else:
            K.op(eng, lambda e: e.tensor_scalar(out=out, in0=in0, scalar1=s1, scalar2=s2, op0=op0, op1=op1),
                 reads=reads, writes=writes)

    def stt(out, in0, scalar, in1, op0, op1, reads, writes):
        K.op("dve", lambda e: e.scalar_tensor_tensor(out=out, in0=in0, scalar=scalar, in1=in1, op0=op0, op1=op1),
             reads=reads, writes=writes)

    def rsqrt_to(dst, dst_t, src, src_t, n, scale):
        act(dst[:, 0:n], src[:, 0:n], AF.Ln, [src_t, epsc_t], [dst_t], bias=epsc[:, 0:1], scale=scale)
        act(dst[:, 0:n], dst[:, 0:n], AF.Exp, [dst_t], [dst_t], scale=-0.5)

    def rsqrt_cols(n, scale):
        act(rs16[:, 0:n], st16[:, 0:n], AF.Ln, [st16_t, epsc_t], [rs16_t], bias=epsc[:, 0:1], scale=scale)
        act(rs16[:, 0:n], rs16[:, 0:n], AF.Exp, [rs16_t], [rs16_t], scale=-0.5)

    def wview(ap2d, kk):
        return ap2d.rearrange("(k p) c -> p k c", p=128)

    def layer_blocks(l):
        win = wview(w_in[l], 8)
        out = []

        def blk(key, c0, n):
            out.append((key, [(0, win[:, :, c0:c0 + n], 8, n)], 8, n))
        blk("A_u", 0, 512); blk("A_z", 1024, 512); blk("A_v", 512, 512)
        blk("B_q", 1536, 512); blk("B_k", 2048, 512); blk("B_v", 2560, 512); blk("B_z", 3072, 512)
        blk("C_qk", 3584, 512); blk("C_lr", 4608, 16); blk("C_z", 4624, 512); blk("C_v", 4096, 512)
        for c in range(4):
            parts = []
            for qi, base in enumerate((5648, 6160, 5136, 6672)):
                parts.append((qi * 128, win[:, :, base + c * 128: base + (c + 1) * 128], 8, 128))
            out.append(("D_%d" % c, parts, 8, 512))
        for c2 in range(2):
            for i in range(4):
                out.append(("G_%d_%d" % (i, c2), [(0, wview(w_mg[l, i], 8)[:, :, c2 * 512:(c2 + 1) * 512], 8, 512)], 8, 512))
                out.append(("P_%d_%d" % (i, c2), [(0, wview(w_bo[l, i], 4)[:, :, c2 * 512:(c2 + 1) * 512], 4, 512)], 4, 512))
        for half in range(2):
            out.append(("O_%d" % half, [(0, wview(w_o[l], 8)[:, :, half * 512:(half + 1) * 512], 8, 512)], 8, 512))
        return out

    LB = [layer_blocks(l) for l in range(NL)]
    NB = len(LB[0])
    wsc = nc.dram_tensor("wsc", [NL * NB, 128, 4096], BF16).ap()
    wsc_t = [[Tr() for _ in range(NB)] for _ in range(NL)]
    conv_hist = []
    conv_pos = [0 for _ in range(NL)]

    def wsc_view(l, bi, kk, ntot):
        return wsc[l * NB + bi][:, 0:kk * ntot].rearrange("p (k c) -> p k c", k=kk)

    def convert_blocks(l, count):
        for _ in range(count):
            bi = conv_pos[l]
            if bi >= NB:
                return
            conv_pos[l] += 1
            key, parts, kk, ntot = LB[l][bi]
            dst3 = wsc_view(l, bi, kk, ntot)
            for (c0, src, kk_, n) in parts:
                thr = [conv_hist[-4]] if len(conv_hist) >= 4 else []
                tmp = Tr()
                K.dma("pool", dst3[:, :, c0:c0 + n], src, reads=thr, writes=[wsc_t[l][bi], tmp])
                conv_hist.append(tmp)

    wseq = []
    for l in range(NL):
        for gi in range(NG):
            for bi, (key, parts, kk, ntot) in enumerate(LB[l]):
                wseq.append((key, l, bi, kk, ntot))

    WP = Pool("W", [t[:, :, :] for t in wring])
    wstate = {"issued": 0, "cur": 0, "slot": {}}

    def w_ensure(upto):
        while wstate["issued"] <= upto and wstate["issued"] < len(wseq) and WP.free_list:
            i = wstate["issued"]
            s = WP.alloc()
            key, l_, bi, kk, ntot = wseq[i]
            K.dma("sp", WP.ap(s)[:, 0:kk, 0:ntot], wsc_view(l_, bi, kk, ntot), reads=[wsc_t[l_][bi]], writes=[WP.tr(s)])
            wstate["slot"][i] = s
            wstate["issued"] += 1

    def w_get(key):
        i = wstate["cur"]
        assert wseq[i][0] == key, (wseq[i][0], key)
        w_ensure(i + 2)
        s = wstate["slot"][i]
        return WP.ap(s), WP.tr(s)

    def w_done():
        i = wstate["cur"]
        WP.free(wstate["slot"].pop(i))
        wstate["cur"] += 1
        w_ensure(wstate["cur"] + 2)

    def init_const():
        K.dma("pool", ident_bf[:, :], cst["c_ident"], writes=[ident_t])
        K.dma("pool", uincl_bf[:, :], cst["c_uincl"], writes=[uinclb_t])
        K.dma("sp", uincl_f[:, :], cst["c_uincl"], writes=[uinclf_t])
        K.dma("pool", lstr_bf[:, :], cst["c_lstrict"], writes=[lstrb_t])
        K.dma("sp", alibi[:, :], cst["c_alibi"], writes=[alibi_t])
        K.dma("sp", cmb[:, :], cst["c_cmb"], writes=[cmb_t])
        for j in range(17):
            K.dma("pool", esel[:, j, :], cst["c_esel"][:, j * 128:(j + 1) * 128], writes=[esel_t])
        K.op("dve", lambda e: e.memset(ones_f[:, :], 1.0), writes=[ones_t])
        K.op("dve", lambda e: e.memset(epsc[:, :], EPS), writes=[epsc_t])
        K.op("dve", lambda e: e.memset(lrT[:, :], 1.0), writes=[lrT_t])
        for i_ in range(2):
            K.op("dve", lambda e: e.memset(AT[i_][:, :, :], 0.0), writes=[AT_t[i_]])
        K.op("dve", lambda e: e.memset(vc[:, :, :, 128:129], 1.0), writes=[vc_t])
        for t in (ident_t, uinclb_t, uinclf_t, lstrb_t, alibi_t, cmb_t, esel_t, ones_t, epsc_t):
            t.ro = True

    def init_R(l):
        K.dma("sp", gbc[:, :], norm_g[l:l + 1, :].broadcast_to([128, D]), writes=[gbc_t])

    def init_layer(l):
        K.dma("sp", lng[:, :], a_ln_g[l:l + 1, :].broadcast_to([128, 512]), writes=[lng_t])
        K.dma("sp", lnb[:, :], a_ln_b[l:l + 1, :].broadcast_to([128, 512]), writes=[lnb_t])
        K.dma("sp", bsf[:, :], a_sb[l:l + 1, :], writes=[bsf_t])
        f = FP.alloc()
        for g in range(4):
            K.dma("sp", Ff(f)[:, g * 128:(g + 1) * 128], a_wsT[l, g], writes=[FP.tr(f)])
        tt(WsT[:, :, :], v3(Ff(f), 4), uincl_f[:, :].unsqueeze(1).broadcast_to([128, 4, 128]), ALU.mult,
           [FP.tr(f), uinclf_t], [WsT_t])
        FP.free(f)
        with nc.allow_non_contiguous_dma(reason="tiny per-layer parameter columns"):
            K.dma("sp", gq_col[:, :], b_qg[l:l + 1, :].rearrange("o d -> d o"), writes=[gq_t])
            K.dma("sp", gk_col[:, :], b_kg[l:l + 1, :].rearrange("o d -> d o"), writes=[gk_t])
            K.dma("sp", og_col[:, :], c_og[l:l + 1, :].rearrange("o d -> d o"), writes=[og_t])
            for j in range(3):
                K.dma("sp", cw[:, j, :], d_cw[l, j:j + 1, :].rearrange("o (c p) -> p (o c)", p=128), writes=[cw_t])
            K.dma("sp", cb[:, :], d_cb[l:l + 1, :].rearrange("o (c p) -> p (o c)", p=128), writes=[cb_t])
            for i in range(4):
                K.dma("sp", bmg[:, i, :], b_mg[l, i:i + 1, :].rearrange("o (f p) -> p (o f)", p=128), writes=[bmg_t])
        ts(gq_col[:, :], gq_col[:, :], 128.0 ** -0.5, None, ALU.mult, None, [gq_t], [gq_t])
        K.dma("pool", w2b[0:16, :], c_w2[l], writes=[w2b_t])
        K.dma("pool", w2b[16:17, :], c_b2[l:l + 1, :], writes=[w2b_t])
        K.op("dve", lambda e: e.memset(Sst[:, :, :], 0.0), writes=[Sst_t])
        K.op("dve", lambda e: e.memset(Sbf[:, :, :], 0.0), writes=[Sbf_t])
        K.op("dve", lambda e: e.memset(uh[:, :, :], 0.0), writes=[uh_t])
        K.op("dve", lambda e: e.memset(kmT[:, :, :], 0.0), writes=[kmT_t])
        K.op("dve", lambda e: e.memset(Gs[:, :, 0:16], -1.0e30), writes=[Gs_t])
        K.op("dve", lambda e: e.memset(Gs[:, :, 16:17], 1.0e30), writes=[Gs_t])

    def proj_fm(wb, wtr, c0, evac, nparts=128, kk=8, rhs_of=None, rhs_tr=None):
        p = PS.alloc()
        for k in range(kk):
            rhs = hT2[cur_h["i"]][:, k, :] if rhs_of is None else rhs_of(k)
            mm(Pf(p)[0:nparts, 0:TG], wb[:, k, c0:c0 + nparts], rhs, k == 0, k == kk - 1,
               [wtr, hT2_t[cur_h["i"]] if rhs_tr is None else rhs_tr], PS.tr(p))
        evac(p)
        PS.free(p)

    def proj_tm(wb, wtr, ti, c0, n, evac):
        p = PS.alloc()
        for k in range(8):
            mm(Pf(p)[:, 0:n], hT2[cur_h["i"]][:, k, ti * 128:(ti + 1) * 128], wb[:, k, c0:c0 + n], k == 0, k == 7,
               [wtr, hT2_t[cur_h["i"]]], PS.tr(p))
        evac(p)
        PS.free(p)

    def stage_R(l, gi):
        src = x_d if l == 0 else y_d
        xs = []
        for ti in range(NT):
            T = gi * NT + ti
            h = HP.alloc()
            xs.append(h)
            K.dma("pool", Hf(h), src[T * 128:(T + 1) * 128, :], reads=([ytr[T]] if l > 0 else []), writes=[HP.tr(h)])
            f = FP.alloc()
            act(Fb(f)[:, 0:D], Hf(h), AF.Square, [HP.tr(h)], [FP.tr(f), st16_t], accum_out=st16[:, ti:ti + 1])
            FP.free(f)
        rsqrt_cols(NT, 1.0 / D)
        hbuf = hT2[cur_h["i"]]
        hbuf_t = hT2_t[cur_h["i"]]
        fs = []
        for ti in range(NT):
            h = xs[ti]
            f = FP.alloc()
            stt(Fb(f)[:, 0:D], Hf(h), rs16[:, ti:ti + 1], gbc[:, :], ALU.mult, ALU.mult,
                [HP.tr(h), rs16_t, gbc_t], [FP.tr(f)])
            HP.free(h)
            fs.append(f)

        def pe_part():
            for ti in range(NT):
                f = fs[ti]
                p = PS.alloc()
                for c in range(8):
                    tp(Pb(p)[:, c * 128:(c + 1) * 128], Fb(f)[:, c * 128:(c + 1) * 128], [FP.tr(f)], PS.tr(p))
                K.op("act", lambda e: e.copy(out=hbuf[:, :, ti * 128:(ti + 1) * 128], in_=v3(Pb(p), 8)),
                     reads=[PS.tr(p)], writes=[hbuf_t])
                PS.free(p)
                FP.free(f)
        return pe_part

    def stage_A1(l, gi):
        wb, wtr = w_get("A_u")
        gu = HP.alloc()
        for c in range(4):
            proj_fm(wb, wtr, c * 128, lambda p: act(v3(Hb(gu), 4)[:, c, :], Pf(p)[:, 0:TG], AF.Gelu_apprx_tanh,
                                                    [PS.tr(p)], [HP.tr(gu)]))
        w_done()
        wb, wtr = w_get("A_z")
        sz = HP.alloc()
        for c in range(4):
            proj_fm(wb, wtr, c * 128, lambda p: act(v3(Hb(sz), 4)[:, c, :], Pf(p)[:, 0:TG], AF.Silu,
                                                    [PS.tr(p)], [HP.tr(sz)]))
        w_done()
        tt(Hb(gu), Hb(gu), Hb(sz), ALU.mult, [HP.tr(gu), HP.tr(sz)], [HP.tr(gu)])
        HP.free(sz)
        wb, wtr = w_get("A_v")
        gv = []
        for ti in range(NT):
            f = FP.alloc()
            gv.append(f)
            proj_tm(wb, wtr, ti, 0, 512, lambda p: act(Ff(f), Pf(p), AF.Gelu_apprx_tanh, [PS.tr(p)], [FP.tr(f)]))
            K.op("dve", lambda e: e.bn_stats(out=bnst[:, ti, :], in_=Ff(f)), reads=[FP.tr(f)], writes=[bnst_t])
            K.op("dve", lambda e: e.bn_aggr(out=bnmv[:, ti, :], in_=bnst[:, ti, :]), reads=[bnst_t], writes=[bnmv_t])
        w_done()
        K.op("dve", lambda e: e.tensor_copy(out=st16a[:, 0:NT], in_=bnmv[:, :, 1]), reads=[bnmv_t], writes=[st16a_t])
        rsqrt_to(rs16a, rs16a_t, st16a, st16a_t, NT, 1.0)
        vln = []
        for ti in range(NT):
            f = gv[ti]
            ts(Ff(f), Ff(f), bnmv[:, ti, 0:1], rs16a[:, ti:ti + 1], ALU.subtract, ALU.mult,
               [FP.tr(f), bnmv_t, rs16a_t], [FP.tr(f)])
            tt(Ff(f), Ff(f), lng[:, :], ALU.mult, [FP.tr(f), lng_t], [FP.tr(f)])
            f2 = FP.alloc()
            tt(Fb(f2)[:, 0:512], Ff(f), lnb[:, :], ALU.add, [FP.tr(f), lnb_t], [FP.tr(f2)])
            FP.free(f)
            vln.append(f2)
        return gu, vln

    def stage_A2(l, gi, gu, vln):
        for ti in range(NT):
            f2 = vln[ti]
            p = PS.alloc()
            for g in range(4):
                mm(Pf(p)[:, g * 128:(g + 1) * 128], Fb(f2)[:, g * 128:(g + 1) * 128], WsT[:, g, :], True, False,
                   [FP.tr(f2), WsT_t], PS.tr(p))
                mm(Pf(p)[:, g * 128:(g + 1) * 128], ones_f[0:1, 0:128], bsf[0:1, g * 128:(g + 1) * 128], False, True,
                   [ones_t, bsf_t], PS.tr(p))
            tt(yT[0][:, :, ti * 128:(ti + 1) * 128], v3(Pf(p), 4), v3(Hb(gu), 4)[:, :, ti * 128:(ti + 1) * 128], ALU.mult,
               [PS.tr(p), HP.tr(gu)], [yT_t[0]])
            PS.free(p)
            FP.free(f2)
        HP.free(gu)

    def stage_B1(l, gi):
        raws = {}
        for key, ssq, ssq_t in (("B_q", st16q, st16q_t), ("B_k", st16k, st16k_t)):
            wb, wtr = w_get(key)
            hs = [HP.alloc(), HP.alloc()]
            raws[key] = hs
            for ti in range(NT):
                dst = v3(Hf(hs[ti // 2]), 2)[:, ti % 2, :]
                dtr = HP.tr(hs[ti // 2])
                proj_tm(wb, wtr, ti, 0, 512, lambda p: K.op("act", lambda e: e.copy(out=dst, in_=Pf(p)),
                                                             reads=[PS.tr(p)], writes=[dtr]))
                f2 = FP.alloc()
                tt(Ff(f2), dst, dst, ALU.mult, [dtr], [FP.tr(f2)])
                K.op("dve", lambda e: e.tensor_reduce(out=ssq[:, ti * 4:(ti + 1) * 4], in_=v3(Ff(f2), 4), axis=AX.X, op=ALU.add),
                     reads=[FP.tr(f2)], writes=[ssq_t])
                FP.free(f2)
            w_done()
        rsqrt_to(rs16q, rs16q_t, st16q, st16q_t, 16, 1.0 / 128)
        rsqrt_to(rs16k, rs16k_t, st16k, st16k_t, 16, 1.0 / 128)
        return raws

    def stage_B2(l, gi, raws):
        qT = HP.alloc()
        qTv = v3(Hb(qT), 4)
        wb, wtr = w_get("B_v")
        for ti in range(NT):
            T = gi * NT + ti
            proj_tm(wb, wtr, ti, 0, 512, lambda p: K.op("act", lambda e: e.copy(out=vc[:, T, :, 0:128], in_=v3(Pf(p), 4)),
                                                         reads=[PS.tr(p)], writes=[vc_t]))
        w_done()
        for key, ssq, ssq_t, rsx, rsx_t, gcol, gtr, dst_fn, dst_tr in (
                ("B_q", st16q, st16q_t, rs16q, rs16q_t, gq_col, gq_t,
                 lambda ti: qTv[:, :, ti * 128:(ti + 1) * 128], HP.tr(qT)),
                ("B_k", st16k, st16k_t, rs16k, rs16k_t, gk_col, gk_t,
                 lambda ti: kT[:, :, (gi * NT + ti) * 128:(gi * NT + ti + 1) * 128], kT_t)):
            hs = raws[key]
            for ti in range(NT):
                src = v3(Hf(hs[ti // 2]), 2)[:, ti % 2, :]
                f2 = FP.alloc()
                tt(v3(Fb(f2)[:, 0:512], 4), v3(src, 4), rsx[:, ti * 4:(ti + 1) * 4].unsqueeze(2).broadcast_to([128, 4, 128]),
                   ALU.mult, [HP.tr(hs[ti // 2]), rsx_t], [FP.tr(f2)])
                p = PS.alloc()
                for h in range(4):
                    tp(Pb(p)[:, h * 128:(h + 1) * 128], Fb(f2)[:, h * 128:(h + 1) * 128], [FP.tr(f2)], PS.tr(p))
                act(dst_fn(ti), v3(Pb(p)[:, 0:512], 4), AF.Identity, [PS.tr(p), gtr], [dst_tr], scale=gcol[:, 0:1])
                PS.free(p)
                FP.free(f2)
            HP.free(hs[0])
            HP.free(hs[1])
        for bl in range(2):
            B = gi * 2 + bl
            K.op("dve", lambda e: e.tensor_reduce(out=kms[:, :], in_=kT[:, :, B * 256:(B + 1) * 256], axis=AX.X, op=ALU.add),
                 reads=[kT_t], writes=[kms_t])
            ts(kmT[:, :, B], kms[:, :], 1.0 / 256, None, ALU.mult, None, [kms_t], [kmT_t])
        def topk_tile(ti):
            T = gi * NT + ti
            B = T // 2
            par = T % 2
            if B > 0:
                p = PS.alloc()
                for h in range(4):
                    mm(Pf(p)[:, h * 16:(h + 1) * 16], qTv[:, h, ti * 128:(ti + 1) * 128], kmT[:, h, 0:16], True, True,
                       [HP.tr(qT), kmT_t], PS.tr(p))
                K.op("dve", lambda e: e.tensor_copy(out=Gs[:, :, 0:B], in_=v3(Pf(p)[:, 0:64], 4)[:, :, 0:B]),
                     reads=[PS.tr(p)], writes=[Gs_t])
                PS.free(p)
            for h in range(4):
                K.op("dve", lambda e: e.max(out=m8[:, h * 8:(h + 1) * 8], in_=Gs[:, h, 0:16]), reads=[Gs_t], writes=[m8_t])
            for h in range(4):
                ts(msk[:, h, :], Gs[:, h, :], m8[:, h * 8 + 2:h * 8 + 3], None, ALU.is_ge, None, [Gs_t, m8_t], [msk_t])
            for h in range(4):
                ts(Aas[ti][:, h, :], msk[:, h, :], BIG, cmb[:, h * 2 + par:h * 2 + par + 1], ALU.mult, ALU.add,
                   [msk_t, cmb_t], [Aas_t[ti]])

        def at_tile(ti):
            T = gi * NT + ti
            B = T // 2
            par = T % 2
            at = AT[B % 2]
            at_t = AT_t[B % 2]
            p = PS.alloc()
            for h in range(4):
                tp(Pb(p)[0:17, h * 128:(h + 1) * 128], Aas[ti][:, h, :], [Aas_t[ti]], PS.tr(p))
            K.op("act", lambda e: e.copy(out=at[0:17, :, par * 128:(par + 1) * 128], in_=v3(Pb(p)[0:17, 0:512], 4)),
                 reads=[PS.tr(p)], writes=[at_t])
            PS.free(p)

        topk_tile(0)
        topk_tile(1)
        wb, wtr = w_get("B_z")
        szb = HP.alloc()
        for c in range(4):
            proj_fm(wb, wtr, c * 128, lambda p: act(v3(Hb(szb), 4)[:, c, :], Pf(p)[:, 0:TG], AF.Silu,
                                                    [PS.tr(p)], [HP.tr(szb)]))
        w_done()
        at_tile(0)
        at_tile(1)
        topk_tile(2)
        topk_tile(3)
        steps = []
        for bl in range(2):
            for h in range(4):
                for kt in range(2 * (gi * 2 + bl) + 2):
                    steps.append((bl, h, kt))
        LA = 3
        obs = [FP.alloc(), FP.alloc()]
        obvs = [Fb(o).rearrange("p (q h d) -> p q h d", q=2, h=4) for o in obs]
        issued = {}
        accs = {}

        first_b1 = 4 * (2 * (gi * 2) + 2)

        def issue(i):
            if i == first_b1:
                at_tile(2)
                at_tile(3)
            bl, h, kt = steps[i]
            B = gi * 2 + bl
            at, at_t = AT[B % 2], AT_t[B % 2]
            qloc = bl * 256
            j = kt // 2
            qs_ = 0 if kt <= 2 * B else 128
            n = 256 - qs_
            pss = PS.alloc()
            mm(Pf(pss)[:, 0:n], kT[:, h, kt * 128:(kt + 1) * 128], qTv[:, h, qloc + qs_:qloc + 256], True, False,
               [kT_t, HP.tr(qT)], PS.tr(pss))
            jsel = j if j < B else 16
            mm(Pf(pss)[:, 0:n], esel[:, jsel, :], at[:, h, qs_:256], False, True, [esel_t, at_t], PS.tr(pss))
            issued[i] = (pss, n, qs_)

        def finish_block(bl):
            for qt in range(2):
                tcol = (bl * 2 + qt) * 128
                p = PS.alloc()
                for h in range(4):
                    tp(Pb(p)[:, h * 128:(h + 1) * 128], obvs[bl][:, qt, h, :], [FP.tr(obs[bl])], PS.tr(p))
                tt(yT[1][:, :, tcol:tcol + 128], v3(Pb(p)[:, 0:512], 4), v3(Hb(szb), 4)[:, :, tcol:tcol + 128], ALU.mult,
                   [PS.tr(p), HP.tr(szb)], [yT_t[1]])
                PS.free(p)
            FP.free(obs[bl])

        deferred = []
        for i in range(min(LA, len(steps))):
            issue(i)
        for i, (bl, h, kt) in enumerate(steps):
            if i + LA < len(steps):
                issue(i + LA)
            B = gi * 2 + bl
            nkt = 2 * B + 2
            if kt == 0:
                accs[(bl, h)] = [PS.alloc(), PS.alloc()]
            ac = accs[(bl, h)]
            pss, n, qs_ = issued.pop(i)
            pt = PTP.alloc()
            aidx = h * 32 + (2 * B - kt + 1)
            act(PTP.ap(pt)[:, 0:n], Pf(pss)[:, 0:n], AF.Exp, [PS.tr(pss), alibi_t], [PTP.tr(pt)],
                bias=alibi[:, aidx:aidx + 1], scale=1.0)
            PS.free(pss)
            if kt >= 2 * B:
                tt(PTP.ap(pt)[:, 0:128], PTP.ap(pt)[:, 0:128], uincl_bf[:, :], ALU.mult,
                   [PTP.tr(pt), uinclb_t], [PTP.tr(pt)])
            for qt in range(2):
                if kt <= 2 * B + qt:
                    c0 = qt * 128 - qs_
                    mm(Pf(ac[qt])[:, 0:129], PTP.ap(pt)[:, c0:c0 + 128], vc[:, kt, h, :], kt == 0, kt == 2 * B + qt,
                       [PTP.tr(pt), vc_t], PS.tr(ac[qt]))
            PTP.free(pt)
            if kt == nkt - 1:
                for qt in range(2):
                    a_ = ac[qt]
                    K.op("dve", lambda e: e.reciprocal(out=rc2[:, qt:qt + 1], in_=Pf(a_)[:, 128:129]),
                         reads=[PS.tr(a_)], writes=[rc2_t])
                    ts(obvs[bl][:, qt, h, :], Pf(a_)[:, 0:128], rc2[:, qt:qt + 1], None, ALU.mult, None,
                       [PS.tr(a_), rc2_t], [FP.tr(obs[bl])])
                    PS.free(a_)
                del accs[(bl, h)]
                if h == 3:
                    deferred.append((i + LA + 2, bl))
            while deferred and deferred[0][0] <= i:
                finish_block(deferred.pop(0)[1])

        def tail():
            while deferred:
                finish_block(deferred.pop(0)[1])
            HP.free(qT)
            HP.free(szb)
        return tail

    def stage_C(l, gi, dgen=None, pre=None):
        wb, wtr = w_get("C_qk")
        qf = HP.alloc()
        kf = HP.alloc()
        ktm = HP.alloc()
        for c in range(4):
            dst = v3(Hf(qf), 2)[:, c, :] if c < 2 else v3(Hf(kf), 2)[:, c - 2, :]
            dtr = HP.tr(qf) if c < 2 else HP.tr(kf)
            proj_fm(wb, wtr, c * 128, lambda p: K.op("act", lambda e: e.copy(out=dst, in_=Pf(p)[:, 0:TG]),
                                                     reads=[PS.tr(p)], writes=[dtr]))
        for ti in range(NT):
            proj_tm(wb, wtr, ti, 256, 256, lambda p: K.op("act", lambda e: e.copy(out=v3(Hf(ktm), 4)[:, ti, :], in_=Pf(p)[:, 0:256]),
                                                           reads=[PS.tr(p)], writes=[HP.tr(ktm)]))
        w_done()
        if pre is not None:
            pre()
        wb, wtr = w_get("C_lr")
        proj_fm(wb, wtr, 0, lambda p: K.op("act", lambda e: e.copy(out=lrT[0:16, :], in_=Pf(p)[0:16, 0:TG]),
                                           reads=[PS.tr(p)], writes=[lrT_t]), nparts=16)
        w_done()
        hls = []

        def p1a():
            for ti in range(NT):
                tsl = slice(ti * 128, (ti + 1) * 128)
                p = PS.alloc()
                mm(Pf(p)[:, 0:256], lrT[0:17, tsl], w2b[0:17, :], True, True, [lrT_t, w2b_t], PS.tr(p))
                lg = FP.alloc()
                act(Ff(lg)[:, 0:256], Pf(p)[:, 0:256], AF.Exp, [PS.tr(p)], [FP.tr(lg)], scale=-1.0)
                PS.free(p)
                act(Ff(lg)[:, 0:256], Ff(lg)[:, 0:256], AF.Ln, [FP.tr(lg)], [FP.tr(lg)], bias=1.0, scale=1.0)
                hl = FP.alloc()
                hlv = Fb(hl)
                K.op("dve", lambda e: e.tensor_copy(out=hlv[:, 0:256], in_=Ff(lg)[:, 0:256]), reads=[FP.tr(lg)], writes=[FP.tr(hl)])
                tt(hlv[:, 256:512], Ff(lg)[:, 0:256], hlv[:, 0:256], ALU.subtract, [FP.tr(lg), FP.tr(hl)], [FP.tr(hl)])
                FP.free(lg)
                hls.append(hl)

        def p1b():
            for ti in range(NT):
                tsl = slice(ti * 128, (ti + 1) * 128)
                dec2, dec2_t = dec2s[ti], dec2s_t[ti]
                qtl, qtl_t = qtls[ti], qtls_t[ti]
                ktl, ktl_t = ktls[ti], ktls_t[ti]
                kdec, kdec_t = kdecs[ti], kdecs_t[ti]
                hl = hls[ti]
                hlv = Fb(hl)
                p = PS.alloc()
                for c in range(2):
                    mm(Pf(p)[:, c * 128:(c + 1) * 128], hlv[:, c * 128:(c + 1) * 128], uincl_bf[:, :], True, False,
                       [FP.tr(hl), uinclb_t], PS.tr(p))
                    mm(Pf(p)[:, c * 128:(c + 1) * 128], hlv[:, 256 + c * 128:256 + (c + 1) * 128], uincl_bf[:, :], False, True,
                       [FP.tr(hl), uinclb_t], PS.tr(p))
                mm(Pf(p)[:, 256:512], lstr_bf[:, :], hlv[:, 0:256], True, False, [FP.tr(hl), lstrb_t], PS.tr(p))
                mm(Pf(p)[:, 256:512], lstr_bf[:, :], hlv[:, 256:512], False, True, [FP.tr(hl), lstrb_t], PS.tr(p))
                FP.free(hl)
                e1 = FP.alloc()
                e2 = FP.alloc()
                act(Ff(e1)[:, 0:256], Pf(p)[:, 0:256], AF.Exp, [PS.tr(p)], [FP.tr(e1)], scale=-1.0 / 16)
                act(Ff(e1)[:, 256:512], Pf(p)[:, 0:256], AF.Exp, [PS.tr(p)], [FP.tr(e1)], scale=1.0 / 16)
                act(Ff(e2)[:, 0:256], Pf(p)[:, 256:512], AF.Exp, [PS.tr(p)], [FP.tr(e2)], scale=-1.0 / 16)
                act(dec2[:, :], v3(Pf(p)[:, 0:256], 2)[:, :, 127], AF.Exp, [PS.tr(p)], [dec2_t], scale=-1.0 / 16)
                PS.free(p)
                stt(qtl[:, :, :], v3(Hf(qf), 2)[:, :, tsl], 0.125, v3(Ff(e1)[:, 0:256], 2), ALU.mult, ALU.mult,
                    [HP.tr(qf), FP.tr(e1)], [qtl_t])
                tt(ktl[:, :, :], v3(Hf(kf), 2)[:, :, tsl], v3(Ff(e1)[:, 256:512], 2), ALU.mult,
                   [HP.tr(kf), FP.tr(e1)], [ktl_t])
                tt(kdec[:, :], v3(Hf(ktm), 4)[:, ti, :], Ff(e2)[:, 0:256], ALU.mult, [HP.tr(ktm), FP.tr(e2)], [kdec_t])
                FP.free(e1)
                FP.free(e2)

        def p1c():
            for ti in range(NT):
                qtl, qtl_t = qtls[ti], qtls_t[ti]
                ktl, ktl_t = ktls[ti], ktls_t[ti]
                attT, attT_t = attTs[ti], attTs_t[ti]
                pa = [PS.alloc(), PS.alloc()]
                for h in range(4):
                    c, po = h // 2, 64 * (h % 2)
                    mm(Pf(pa[h % 2])[:, c * 128:(c + 1) * 128], ktl[po:po + 64, c, :], qtl[po:po + 64, c, :], True, True,
                       [ktl_t, qtl_t], PS.tr(pa[h % 2]))
                for r in range(2):
                    tt(attT[:, r::2, :], v3(Pf(pa[r])[:, 0:256], 2), uincl_f[:, :].unsqueeze(1).broadcast_to([128, 2, 128]), ALU.mult,
                       [PS.tr(pa[r]), uinclf_t], [attT_t])
                    PS.free(pa[r])


        p1a()
        wb, wtr = w_get("C_z")
        szc = HP.alloc()
        for c in range(4):
            proj_fm(wb, wtr, c * 128, lambda p: act(v3(Hb(szc), 4)[:, c, :], Pf(p)[:, 0:TG], AF.Silu,
                                                    [PS.tr(p)], [HP.tr(szc)]))
        w_done()
        p1b()
        wb, wtr = w_get("C_v")
        vg = HP.alloc()
        for ti in range(NT):
            proj_tm(wb, wtr, ti, 0, 512, lambda p: K.op("act", lambda e: e.copy(out=v3(Hb(vg), 4)[:, ti, :], in_=Pf(p)),
                                                         reads=[PS.tr(p)], writes=[HP.tr(vg)]))
        w_done()
        vgv = v3(Hb(vg), 4)
        p1c()

        def finish_tile(ti, sq):
            tsl = slice(ti * 128, (ti + 1) * 128)
            p = PS.alloc()
            for h in range(4):
                tp(Pb(p)[:, h * 128:(h + 1) * 128], Fb(sq)[:, h * 128:(h + 1) * 128], [FP.tr(sq)], PS.tr(p))
            stt(yT[2][:, :, tsl], v3(Pb(p)[:, 0:512], 4), og_col[:, 0:1], v3(Hb(szc), 4)[:, :, tsl], ALU.mult, ALU.mult,
                [PS.tr(p), og_t, HP.tr(szc)], [yT_t[2]])
            PS.free(p)
            FP.free(sq)

        pend = None
        if dgen is not None:
            next(dgen, None)
        for ti in range(NT):
            dec2, dec2_t = dec2s[ti], dec2s_t[ti]
            qtl, qtl_t = qtls[ti], qtls_t[ti]
            kdec, kdec_t = kdecs[ti], kdecs_t[ti]
            attT, attT_t = attTs[ti], attTs_t[ti]
            po_ = PS.alloc()
            for h in range(4):
                c, po = h // 2, 64 * (h % 2)
                mm(Pf(po_)[:, h * 128:(h + 1) * 128], attT[:, h, :], vgv[:, ti, h * 128:(h + 1) * 128], True, False,
                   [attT_t, HP.tr(vg)], PS.tr(po_))
                mm(Pf(po_)[:, h * 128:(h + 1) * 128], qtl[po:po + 64, c, :], Sbf[po:po + 64, c, (h % 2) * 128:(h % 2 + 1) * 128],
                   False, True, [qtl_t, Sbf_t], PS.tr(po_))
            p = PS.alloc()
            for c in range(2):
                mm(Pf(p)[:, c * 256:(c + 1) * 256], kdec[:, c * 128:(c + 1) * 128], vgv[:, ti, c * 256:(c + 1) * 256], True, True,
                   [kdec_t, HP.tr(vg)], PS.tr(p))
            for c in range(2):
                stt(Sst[:, c, :], Sst[:, c, :], dec2[:, c:c + 1], Pf(p)[:, c * 256:(c + 1) * 256], ALU.mult, ALU.add,
                    [Sst_t, dec2_t, PS.tr(p)], [Sst_t])
            PS.free(p)
            K.op("act", lambda e: e.copy(out=Sbf[:, :, :], in_=Sst[:, :, :]), reads=[Sst_t], writes=[Sbf_t])
            if dgen is not None and ti < NT - 1:
                next(dgen, None)
            if pend is not None:
                finish_tile(*pend)
            osb = FP.alloc()
            K.op("act", lambda e: e.copy(out=Ff(osb), in_=Pf(po_)), reads=[PS.tr(po_)], writes=[FP.tr(osb)])
            PS.free(po_)
            sq = FP.alloc()
            tt(Ff(sq), Ff(osb), Ff(osb), ALU.mult, [FP.tr(osb)], [FP.tr(sq)])
            K.op("dve", lambda e: e.tensor_reduce(out=st16[:, 0:4], in_=v3(Ff(sq), 4), axis=AX.X, op=ALU.add),
                 reads=[FP.tr(sq)], writes=[st16_t])
            rsqrt_cols(4, 1.0 / 128)
            tt(v3(Fb(sq)[:, 0:512], 4), v3(Ff(osb), 4), rs16[:, 0:4].unsqueeze(2).broadcast_to([128, 4, 128]), ALU.mult,
               [FP.tr(osb), rs16_t], [FP.tr(sq)])
            FP.free(osb)
            pend = (ti, sq)
        for h_ in (qf, kf, ktm, vg):
            HP.free(h_)

        def ctail():
            finish_tile(*pend)
            HP.free(szc)
        return ctail

    def stage_D(l, gi):
        for c in range(4):
            wb, wtr = w_get("D_%d" % c)
            ps4 = []
            for qi in range(4):
                p = PS.alloc()
                for k in range(8):
                    mm(Pf(p)[:, 0:TG], wb[:, k, qi * 128:(qi + 1) * 128], hT2[cur_h["i"]][:, k, :], k == 0, k == 7, [wtr, hT2_t[cur_h["i"]]], PS.tr(p))
                ps4.append(p)
            w_done()
            p_cg, p_xin, p_bg, p_z = ps4
            u = FP.alloc()
            K.op("act", lambda e: e.copy(out=Ff(u), in_=Pf(p_cg)), reads=[PS.tr(p_cg)], writes=[FP.tr(u)])
            PS.free(p_cg)
            tt(Ff(u), Ff(u), Pf(p_xin), ALU.mult, [FP.tr(u), PS.tr(p_xin)], [FP.tr(u)])
            PS.free(p_xin)
            y = FP.alloc()
            ts(Ff(y), Ff(u), cw[:, 2, c:c + 1], cb[:, c:c + 1], ALU.mult, ALU.add, [FP.tr(u), cw_t, cb_t], [FP.tr(y)])
            stt(Ff(y)[:, 1:512], Ff(u)[:, 0:511], cw[:, 1, c:c + 1], Ff(y)[:, 1:512], ALU.mult, ALU.add,
                [FP.tr(u), cw_t, FP.tr(y)], [FP.tr(y)])
            stt(Ff(y)[:, 2:512], Ff(u)[:, 0:510], cw[:, 0, c:c + 1], Ff(y)[:, 2:512], ALU.mult, ALU.add,
                [FP.tr(u), cw_t, FP.tr(y)], [FP.tr(y)])
            stt(Ff(y)[:, 0:1], uh[:, c, 1:2], cw[:, 1, c:c + 1], Ff(y)[:, 0:1], ALU.mult, ALU.add,
                [uh_t, cw_t, FP.tr(y)], [FP.tr(y)])
            stt(Ff(y)[:, 0:2], uh[:, c, 0:2], cw[:, 0, c:c + 1], Ff(y)[:, 0:2], ALU.mult, ALU.add,
                [uh_t, cw_t, FP.tr(y)], [FP.tr(y)])
            K.op("dve", lambda e: e.tensor_copy(out=uh[:, c, :], in_=Ff(u)[:, 510:512]), reads=[FP.tr(u)], writes=[uh_t])
            FP.free(u)
            sz = FP.alloc()
            act(Ff(sz), Pf(p_z), AF.Silu, [PS.tr(p_z)], [FP.tr(sz)])
            PS.free(p_z)
            tt(Ff(y), Ff(y), Ff(sz), ALU.mult, [FP.tr(y), FP.tr(sz)], [FP.tr(y)])
            FP.free(sz)
            tt(yT[3][:, c, :], Ff(y), Pf(p_bg), ALU.mult, [FP.tr(y), PS.tr(p_bg)], [yT_t[3]])
            PS.free(p_bg)
            FP.free(y)
            yield c

    def stage_M(l, gi, mid=None):
        mT = [HP.alloc(), HP.alloc()]
        for c2 in range(2):
            ma = [HP.alloc(), HP.alloc()]

            def macc(f):
                return v3(Hf(ma[f // 2]), 2)[:, f % 2, :], HP.tr(ma[f // 2])
            for i in range(4):
                wb, wtr = w_get("G_%d_%d" % (i, c2))
                gf = [FP.alloc(), FP.alloc()]
                gh = HP.alloc()
                gaps = [Ff(gf[0]), Ff(gf[1]), v3(Hf(gh), 2)[:, 0, :], v3(Hf(gh), 2)[:, 1, :]]
                gtrs = [FP.tr(gf[0]), FP.tr(gf[1]), HP.tr(gh), HP.tr(gh)]
                for f in range(4):
                    fidx = c2 * 4 + f
                    proj_fm(wb, wtr, f * 128, lambda p: act(gaps[f], Pf(p)[:, 0:TG], AF.Sigmoid, [PS.tr(p), bmg_t], [gtrs[f]],
                                                            bias=bmg[:, i, fidx:fidx + 1], scale=1.0))
                w_done()
                wb, wtr = w_get("P_%d_%d" % (i, c2))
                for f in range(4):
                    gap, gtr_ = gaps[f], gtrs[f]
                    mac, mtr = macc(f)

                    def ev(p):
                        if i == 0:
                            tt(mac, gap, Pf(p)[:, 0:TG], ALU.mult, [gtr_, PS.tr(p)], [mtr])
                        else:
                            tt(gap, gap, Pf(p)[:, 0:TG], ALU.mult, [gtr_, PS.tr(p)], [gtr_])
                            if i < 3:
                                tt(mac, mac, gap, ALU.add, [mtr, gtr_], [mtr])
                            else:
                                tt(v3(Hb(mT[c2]), 4)[:, f, :], mac, gap, ALU.add, [mtr, gtr_], [HP.tr(mT[c2])])
                    proj_fm(wb, wtr, f * 128, ev, kk=4, rhs_of=lambda k: yT[i][:, k, :], rhs_tr=yT_t[i])
                FP.free(gf[0])
                FP.free(gf[1])
                HP.free(gh)
                w_done()
                if mid is not None and c2 == 0 and i == 1:
                    mid()
            HP.free(ma[0])
            HP.free(ma[1])
        return mT

    def stage_O(l, gi, mT):
        for half in range(2):
            wb, wtr = w_get("O_%d" % half)
            src = x_d if l == 0 else y_d
            for ti in range(NT):
                T = gi * NT + ti
                xs = FP.alloc()
                K.dma("pool", Ff(xs), src[T * 128:(T + 1) * 128, half * 512:(half + 1) * 512],
                      reads=([ytr[T]] if l > 0 else []), writes=[FP.tr(xs)])
                p = PS.alloc()
                for f in range(8):
                    mm(Pf(p), v3(Hb(mT[f // 4]), 4)[:, f % 4, ti * 128:(ti + 1) * 128], wb[:, f, 0:512], f == 0, f == 7,
                       [HP.tr(mT[f // 4]), wtr], PS.tr(p))
                tt(Ff(xs), Ff(xs), Pf(p), ALU.add, [FP.tr(xs), PS.tr(p)], [FP.tr(xs)])
                PS.free(p)
                K.dma("sp", y_d[T * 128:(T + 1) * 128, half * 512:(half + 1) * 512], Ff(xs), reads=[FP.tr(xs)], writes=[ytr[T]])
                FP.free(xs)
            w_done()
        HP.free(mT[0])
        HP.free(mT[1])

    skp = sb("skp", [128, 16], BF16); skp_t = Tr()

    def skip_blocks(keys):
        for key in keys:
            wb, wtr = w_get(key)
            K.op("dve", lambda e: e.tensor_copy(out=skp[:, :], in_=wb[:, 0, 0:16]), reads=[wtr], writes=[skp_t])
            w_done()

    init_const()
    gcount = 0
    r_done = False
    for l in range(NL):
        init_layer(l)
        cur_h["i"] = gcount % 2
        if "R" in stages and not r_done:
            init_R(l)
            r0 = stage_R(l, 0)
            if l == 0:
                convert_blocks(0, NB)
            r0()
        elif l == 0:
            convert_blocks(0, NB)
        r_done = False
        for gi in range(NG):
            cur_h["i"] = gcount % 2
            full = all(c in stages for c in "ABCD")
            ctail = None
            if full:
                gu, vln = stage_A1(l, gi)
                raws = stage_B1(l, gi)
                stage_A2(l, gi, gu, vln)
                btail = stage_B2(l, gi, raws)
                dgen = stage_D(l, gi)
                ctail = stage_C(l, gi, dgen, btail)
                for _ in dgen:
                    pass
            else:
                if "A" in stages:
                    gu, vln = stage_A1(l, gi)
                    stage_A2(l, gi, gu, vln)
                else:
                    skip_blocks(["A_u", "A_z", "A_v"])
                if "B" in stages:
                    raws = stage_B1(l, gi)
                    stage_B2(l, gi, raws)()
                else:
                    skip_blocks(["B_q", "B_k", "B_v", "B_z"])
                if "C" in stages:
                    stage_C(l, gi)()
                else:
                    skip_blocks(["C_qk", "C_lr", "C_z", "C_v"])
                if "D" in stages:
                    for _ in stage_D(l, gi):
                        pass
                else:
                    skip_blocks(["D_0", "D_1", "D_2", "D_3"])
            if dbg_d is not None and l == 0 and gi == dbg_group:
                for i in range(4):
                    K.dma("pool", dbg_d[i], yT[i][:, :, :].rearrange("p a b -> p (a b)"), reads=[yT_t[i]])
            if "R" in stages and (gi + 1 < NG or l + 1 < NL):
                cur_h["i"] = (gcount + 1) % 2
                if gi + 1 < NG:
                    r_pe = stage_R(l, gi + 1)
                else:
                    init_R(l + 1)
                    r_pe = stage_R(l + 1, 0)
                    r_done = True
                cur_h["i"] = gcount % 2
            else:
                r_pe = None
            if "M" in stages:
                mT = stage_M(l, gi, ctail)
            else:
                if ctail is not None:
                    ctail()
                skip_blocks(["%s_%d_%d" % (a, i, c2) for c2 in range(2) for i in range(4) for a in ("G", "P")])
                mT = [HP.alloc(), HP.alloc()]
            if r_pe is not None:
                r_pe()
            if "O" in stages:
                stage_O(l, gi, mT)
            else:
                skip_blocks(["O_0", "O_1"])
                HP.free(mT[0]); HP.free(mT[1])
            if l + 1 < NL:
                convert_blocks(l + 1, -(-NB // NG))
            gcount += 1
    assert wstate["cur"] == len(wseq)
    assert all(conv_pos[l_] == NB for l_ in range(NL)), conv_pos
    K.wait_all("sp", ytr + yT_t)
    if needed is None:
        return {k: sorted(v) for k, v in K.waited.items() if k in K.E}
    print("sbuf bytes remaining", nc.sbuf_bytes_remaining)
    print("kernel build: ops=%d waits=%d incs=%d" % (K.n_ops, K.n_waits, sum(len(v) for v in needed.values())))
    return nc


_NC_CACHE = {}


def _prep_shared(inputs):
    sh = {}
    for k in ("norm_g", "w_in", "a_ln_g", "a_ln_b", "b_q_norm_g", "b_k_norm_g", "c_gate_w2", "c_gate_b",
              "c_out_norm_g", "d_conv_w", "d_conv_b", "w_branch_out", "w_merge_gate", "b_merge_gate", "w_out"):
        sh[k] = np.ascontiguousarray(np.asarray(inputs[k], dtype=np.float32))
    sh["a_wsT"] = np.ascontiguousarray(np.transpose(np.asarray(inputs["a_spatial_w"], np.float32), (0, 1, 3, 2)))
    sh["a_spatial_b"] = np.ascontiguousarray(np.asarray(inputs["a_spatial_b"], np.float32).reshape(2, 512))
    sh.update(make_consts())
    return sh


def kernel(**inputs):
    x = np.asarray(inputs["x"], dtype=np.float32)
    nb = x.shape[0]
    if "nc" not in _NC_CACHE:
        _NC_CACHE["nc"] = build()
    nc = _NC_CACHE["nc"]
    sh = _prep_shared(inputs)
    in_maps = []
    for b in range(nb):
        m = dict(sh)
        m["x"] = np.ascontiguousarray(x[b])
        in_maps.append(m)
    res = run_bass_kernel_spmd(nc, in_maps, core_ids=list(range(nb)))
    out = np.stack([np.asarray(r["y"], dtype=np.float32) for r in res.results], axis=0)
    return out
```
